# Optimizing a Trainium2 kernel written in Bass

```python
import math
import jax, jax.numpy as jnp
from jax import lax
import numpy as np


D_MODEL = 2048
BATCH = 1
SEQ = 16384
DEPTH = 2
DEC_BATCH = 4
DEC_SEQ = 4096
PAST_LEN = 128

GRID_W = 64
Q_BLOCK = 128
HEAD_DIM = 128
ROPE_THETA = 500000.0
DIFF_HEADS = 8
DIFF_QK_DIM = HEAD_DIM // 2
DIFF_V_DIM = HEAD_DIM
DIFF_ROT = DIFF_QK_DIM // 4
NA_HEADS = 8
NA_DIM = HEAD_DIM
NA_KH = 8
NA_KW = 16
MLA_HEADS = 8
MLA_Q_RANK = 512
MLA_KV_RANK = 256
MLA_NOPE = 128
MLA_ROPE = 64
MLA_V = 128
MLA_THETA = 10000.0
GQA_HEADS = 8
GQA_KV_HEADS = 2
GQA_DIM = HEAD_DIM
AXIAL_THETA = 10000.0
N_GROUPS = 4
EXPERTS_PER_GROUP = 8
N_EXPERTS = N_GROUPS * EXPERTS_PER_GROUP
TOP_K = 2
D_EXPERT = 512
MOE_BLOCK = 128
DN_ALPHA = (2 * DEPTH) ** 0.25
DN_BETA = (8 * DEPTH) ** -0.25
LN_EPS = 1e-5
RMS_EPS = 1e-6
N_EVEN = (DEPTH + 1) // 2
N_ODD = DEPTH // 2
EVEN_IN = 3 * DIFF_HEADS * HEAD_DIM + 3 * NA_HEADS * NA_DIM
EVEN_OUT = DIFF_HEADS * DIFF_V_DIM + NA_HEADS * NA_DIM
ODD_IN = MLA_Q_RANK + MLA_KV_RANK + MLA_ROPE + (GQA_HEADS + 2 * GQA_KV_HEADS) * GQA_DIM
ODD_OUT = MLA_HEADS * MLA_V + GQA_HEADS * GQA_DIM

kernel_name = "hybrid_diffattn_natten_mla_gqa_hmoe_encoder"


def _rms_norm(x, g, eps=RMS_EPS):
    xf = x.astype(jnp.float32)
    y = xf * lax.rsqrt(jnp.mean(xf * xf, axis=-1, keepdims=True) + eps)
    return (y * g.astype(jnp.float32)).astype(x.dtype)


def _layer_norm(x, g, b, eps=LN_EPS):
    xf = x.astype(jnp.float32)
    mu = jnp.mean(xf, axis=-1, keepdims=True)
    xc = xf - mu
    var = jnp.mean(xc * xc, axis=-1, keepdims=True)
    y = xc * lax.rsqrt(var + eps) * g.astype(jnp.float32) + b.astype(jnp.float32)
    return y.astype(x.dtype)


def _rope_cos_sin(pos, dim, theta):
    inv = theta ** (-jnp.arange(0, dim, 2, dtype=jnp.float32) / dim)
    ang = pos.astype(jnp.float32)[:, None] * inv[None, :]
    return jnp.cos(ang), jnp.sin(ang)


def _rotate(x, cos, sin):
    shape = (cos.shape[0],) + (1,) * (x.ndim - 3) + (cos.shape[1],)
    c = cos.reshape(shape)
    s = sin.reshape(shape)
    xf = x.astype(jnp.float32)
    x1, x2 = jnp.split(xf, 2, axis=-1)
    return jnp.concatenate([x1 * c - x2 * s, x2 * c + x1 * s], axis=-1).astype(x.dtype)


def _partial_rope(x, cos, sin, rot):
    return jnp.concatenate([_rotate(x[..., :rot], cos, sin), x[..., rot:]], axis=-1)


def _axial_rope(x, cos_r, sin_r, cos_c, sin_c):
    half = x.shape[-1] // 2
    return jnp.concatenate([_rotate(x[..., :half], cos_r, sin_r),
                            _rotate(x[..., half:], cos_c, sin_c)], axis=-1)


def _sweep_queries(fn, *qs):
    B, S = qs[0].shape[:2]
    nb = S // Q_BLOCK
    blocks = tuple(jnp.moveaxis(q.reshape((B, nb, Q_BLOCK) + q.shape[2:]), 1, 0) for q in qs)
    out = lax.map(lambda a: fn(*a), blocks)
    out = jnp.moveaxis(out, 0, 1)
    return out.reshape((B, S) + out.shape[3:])


def _diff_attention(q, k, v, lam, subln_g, lambda_init):
    scale = DIFF_QK_DIM ** -0.5

    def blk(qb):
        s = jnp.einsum('bqhmd,bkhmd->bhmqk', qb, k).astype(jnp.float32) * scale
        p = jax.nn.softmax(s, axis=-1)
        a = p[:, :, 0] - lam * p[:, :, 1]
        return jnp.einsum('bhqk,bkhe->bqhe', a.astype(v.dtype), v)

    o = _sweep_queries(blk, q)
    return _rms_norm(o, subln_g, 1e-5) * (1.0 - lambda_init)


def _neighbourhood_attention(q, k, v, rpb, rows):
    B, S, H, d = q.shape
    kh = min(NA_KH, rows)
    qg = q.reshape(B, rows, GRID_W, H, d)
    kg = k.reshape(B, rows, GRID_W, H, d)
    vg = v.reshape(B, rows, GRID_W, H, d)
    cols = np.arange(GRID_W)
    col_start = np.clip(cols - NA_KW // 2, 0, GRID_W - NA_KW)
    col_idx = col_start[:, None] + np.arange(NA_KW)[None, :]
    dc = jnp.asarray(col_idx - cols[:, None] + (NA_KW - 1))
    scale = d ** -0.5

    def row(r):
        rs = jnp.clip(r - kh // 2, 0, rows - kh)
        kr = lax.dynamic_slice_in_dim(kg, rs, kh, axis=1)
        vr = lax.dynamic_slice_in_dim(vg, rs, kh, axis=1)
        kn = jnp.take(kr, col_idx, axis=2)
        vn = jnp.take(vr, col_idx, axis=2)
        qr = lax.dynamic_index_in_dim(qg, r, axis=1, keepdims=False)
        dr = rs + jnp.arange(kh) - r + (NA_KH - 1)
        bias = rpb[:, dr[None, :, None], dc[:, None, :]]
        s = jnp.einsum('bwhd,biwjhd->bhwij', qr, kn).astype(jnp.float32) * scale
        s = s + bias.astype(jnp.float32)[None]
        p = jax.nn.softmax(s.reshape(B, H, GRID_W, kh * NA_KW), axis=-1).reshape(s.shape)
        return jnp.einsum('bhwij,biwjhd->bwhd', p.astype(v.dtype), vn)

    o = lax.map(row, jnp.arange(rows))
    return jnp.moveaxis(o, 0, 1).reshape(B, S, H, d)


def _mla(q_c, kv_c, k_pe, q_norm, w_q_up, kv_norm, w_kv_up, cos, sin):
    B, S, _ = q_c.shape
    q = (_rms_norm(q_c, q_norm) @ w_q_up).reshape(B, S, MLA_HEADS, MLA_NOPE + MLA_ROPE)
    q_nope = q[..., :MLA_NOPE]
    q_pe = _rotate(q[..., MLA_NOPE:], cos, sin)
    kv = (_rms_norm(kv_c, kv_norm) @ w_kv_up).reshape(B, S, MLA_HEADS, MLA_NOPE + MLA_V)
    k_nope = kv[..., :MLA_NOPE]
    v = kv[..., MLA_NOPE:]
    k_pe = _rotate(k_pe, cos, sin)
    scale = (MLA_NOPE + MLA_ROPE) ** -0.5

    def blk(qn, qp):
        s = (jnp.einsum('bqhd,bkhd->bhqk', qn, k_nope)
             + jnp.einsum('bqhr,bkr->bhqk', qp, k_pe)).astype(jnp.float32) * scale
        p = jax.nn.softmax(s, axis=-1)
        return jnp.einsum('bhqk,bkhd->bqhd', p.astype(v.dtype), v)

    o = _sweep_queries(blk, q_nope, q_pe)
    return o.reshape(B, S, MLA_HEADS * MLA_V)


def _gqa_axial(q, k, v, q_norm, k_norm, cos_r, sin_r, cos_c, sin_c):
    B, S = q.shape[:2]
    q = _axial_rope(_rms_norm(q, q_norm), cos_r, sin_r, cos_c, sin_c)
    k = _axial_rope(_rms_norm(k, k_norm), cos_r, sin_r, cos_c, sin_c)
    group = GQA_HEADS // GQA_KV_HEADS
    q = q.reshape(B, S, GQA_KV_HEADS, group, GQA_DIM)
    scale = GQA_DIM ** -0.5

    def blk(qb):
        s = jnp.einsum('bqngd,bknd->bngqk', qb, k).astype(jnp.float32) * scale
        p = jax.nn.softmax(s, axis=-1)
        return jnp.einsum('bngqk,bknd->bqngd', p.astype(v.dtype), v)

    o = _sweep_queries(blk, q)
    return o.reshape(B, S, GQA_HEADS * GQA_DIM)


def _even_mixer(x, w_in, w_out, lam_vec, subln_g, rpb, layer_idx):
    B, S, _ = x.shape
    rows = S // GRID_W
    pos = jnp.arange(S)
    h = x @ w_in
    dq = DIFF_HEADS * HEAD_DIM
    nq = NA_HEADS * NA_DIM
    a_q, a_k, a_v, b_q, b_k, b_v = jnp.split(
        h, [dq, 2 * dq, 3 * dq, 3 * dq + nq, 3 * dq + 2 * nq], axis=-1)
    cos, sin = _rope_cos_sin(pos, DIFF_ROT, ROPE_THETA)
    a_q = _partial_rope(a_q.reshape(B, S, DIFF_HEADS, 2, DIFF_QK_DIM), cos, sin, DIFF_ROT)
    a_k = _partial_rope(a_k.reshape(B, S, DIFF_HEADS, 2, DIFF_QK_DIM), cos, sin, DIFF_ROT)
    a_v = a_v.reshape(B, S, DIFF_HEADS, DIFF_V_DIM)
    lambda_init = 0.8 - 0.6 * math.exp(-0.3 * layer_idx)
    lv = lam_vec.astype(jnp.float32)
    lam = jnp.exp(jnp.sum(lv[0] * lv[1])) - jnp.exp(jnp.sum(lv[2] * lv[3])) + lambda_init
    o_a = _diff_attention(a_q, a_k, a_v, lam, subln_g, lambda_init)
    o_b = _neighbourhood_attention(b_q.reshape(B, S, NA_HEADS, NA_DIM),
                                   b_k.reshape(B, S, NA_HEADS, NA_DIM),
                                   b_v.reshape(B, S, NA_HEADS, NA_DIM), rpb, rows)
    o = jnp.concatenate([o_a.reshape(B, S, -1), o_b.reshape(B, S, -1)], axis=-1)
    return o @ w_out


def _odd_mixer(x, w_in, w_out, q_norm, w_q_up, kv_norm, w_kv_up, g_q_norm, g_k_norm):
    B, S, _ = x.shape
    pos = jnp.arange(S)
    h = x @ w_in
    s1 = MLA_Q_RANK
    s2 = s1 + MLA_KV_RANK
    s3 = s2 + MLA_ROPE
    s4 = s3 + GQA_HEADS * GQA_DIM
    s5 = s4 + GQA_KV_HEADS * GQA_DIM
    q_c, kv_c, k_pe, g_q, g_k, g_v = jnp.split(h, [s1, s2, s3, s4, s5], axis=-1)
    cos, sin = _rope_cos_sin(pos, MLA_ROPE, MLA_THETA)
    o_c = _mla(q_c, kv_c, k_pe, q_norm, w_q_up, kv_norm, w_kv_up, cos, sin)
    cos_r, sin_r = _rope_cos_sin(pos // GRID_W, GQA_DIM // 2, AXIAL_THETA)
    cos_c, sin_c = _rope_cos_sin(pos % GRID_W, GQA_DIM // 2, AXIAL_THETA)
    o_d = _gqa_axial(g_q.reshape(B, S, GQA_HEADS, GQA_DIM),
                     g_k.reshape(B, S, GQA_KV_HEADS, GQA_DIM),
                     g_v.reshape(B, S, GQA_KV_HEADS, GQA_DIM),
                     g_q_norm, g_k_norm, cos_r, sin_r, cos_c, sin_c)
    o = jnp.concatenate([o_c, o_d], axis=-1)
    return o @ w_out


def _grouped_experts(xt, eid, gate, w1, w3, w2):
    T, D = xt.shape
    A = T * TOP_K
    flat_e = eid.reshape(A)
    order = jnp.argsort(flat_e)
    sorted_e = flat_e[order]
    counts = jnp.zeros((N_EXPERTS,), jnp.int32).at[flat_e].add(1)
    padded = (counts + MOE_BLOCK - 1) // MOE_BLOCK * MOE_BLOCK
    pend = jnp.cumsum(padded)
    pstart = pend - padded
    start = jnp.cumsum(counts) - counts
    rank = jnp.arange(A, dtype=jnp.int32) - start[sorted_e]
    dest = pstart[sorted_e] + rank
    nb = -(-A // MOE_BLOCK) + N_EXPERTS
    P = nb * MOE_BLOCK
    buf_tok = jnp.full((P,), T, jnp.int32).at[dest].set((order // TOP_K).astype(jnp.int32))
    block_e = jnp.minimum(jnp.searchsorted(pend, jnp.arange(nb) * MOE_BLOCK, side='right'),
                          N_EXPERTS - 1)
    x_pad = jnp.concatenate([xt, jnp.zeros((1, D), xt.dtype)], axis=0)
    xb = x_pad[buf_tok].reshape(nb, MOE_BLOCK, D)

    def run(args):
        xi, e = args
        hmid = jax.nn.silu(xi @ w1[e]) * (xi @ w3[e])
        return hmid @ w2[e]

    yb = lax.map(run, (xb, block_e)).reshape(P, D)
    y_sorted = yb[dest]
    g_sorted = gate.reshape(A)[order].astype(xt.dtype)
    return jnp.zeros((T, D), xt.dtype).at[order // TOP_K].add(y_sorted * g_sorted[:, None])


def _hier_moe(x, w_group, b_group, w_expert, b_expert, w1, w3, w2):
    B, S, D = x.shape
    T = B * S
    xt = x.reshape(T, D)
    g_logits = (xt @ w_group).astype(jnp.float32) + b_group.astype(jnp.float32)
    g_prob = jax.nn.softmax(g_logits, axis=-1)
    g_sel = jnp.argmax(g_logits, axis=-1)
    e_logits = ((xt @ w_expert).astype(jnp.float32) + b_expert.astype(jnp.float32))
    e_logits = e_logits.reshape(T, N_GROUPS, EXPERTS_PER_GROUP)
    e_in = jnp.take_along_axis(e_logits, g_sel[:, None, None], axis=1)[:, 0]
    top_v, top_i = lax.top_k(e_in, TOP_K)
    gate = jax.nn.softmax(top_v, axis=-1) * jnp.take_along_axis(g_prob, g_sel[:, None], axis=1)
    eid = g_sel[:, None] * EXPERTS_PER_GROUP + top_i
    y = _grouped_experts(xt, eid, gate, w1, w3, w2)
    return y.reshape(B, S, D)


def _trunk(x, ev_w_in, ev_w_out, diff_lambda, diff_subln, na_rpb,
           od_w_in, od_w_out, mla_q_norm, mla_w_q_up, mla_kv_norm, mla_w_kv_up,
           gqa_q_norm, gqa_k_norm, ln1_g, ln1_b, ln2_g, ln2_b,
           moe_w_group, moe_b_group, moe_w_expert, moe_b_expert, moe_w1, moe_w3, moe_w2):
    for l in range(DEPTH):
        i = l // 2
        if l % 2 == 0:
            mix = _even_mixer(x, ev_w_in[i], ev_w_out[i], diff_lambda[i], diff_subln[i],
                              na_rpb[i], l)
        else:
            mix = _odd_mixer(x, od_w_in[i], od_w_out[i], mla_q_norm[i], mla_w_q_up[i],
                             mla_kv_norm[i], mla_w_kv_up[i], gqa_q_norm[i], gqa_k_norm[i])
        x = _layer_norm(DN_ALPHA * x + mix, ln1_g[l], ln1_b[l])
        ff = _hier_moe(x, moe_w_group[l], moe_b_group[l], moe_w_expert[l], moe_b_expert[l],
                       moe_w1[l], moe_w3[l], moe_w2[l])
        x = _layer_norm(DN_ALPHA * x + ff, ln2_g[l], ln2_b[l])
    return x


def _normal(k, shape, scale):
    return jax.random.normal(k, shape, jnp.float32) * scale


def setup_inputs(seed: int = 0) -> dict:
    key = jax.random.key(seed)
    ks = jax.random.split(key, 26)
    D = D_MODEL
    return {
        "x_prompt": _normal(ks[0], (BATCH, SEQ, D), 1.0),
        "x_sample": _normal(ks[1], (DEC_BATCH, DEC_SEQ, D), 1.0),
        "ev_w_in": _normal(ks[2], (N_EVEN, D, EVEN_IN), D ** -0.5),
        "ev_w_out": _normal(ks[3], (N_EVEN, EVEN_OUT, D), DN_BETA * EVEN_OUT ** -0.5),
        "diff_lambda": _normal(ks[4], (N_EVEN, 4, DIFF_QK_DIM), 0.1),
        "diff_subln": 1.0 + _normal(ks[5], (N_EVEN, DIFF_V_DIM), 0.02),
        "na_rpb": _normal(ks[6], (N_EVEN, NA_HEADS, 2 * NA_KH - 1, 2 * NA_KW - 1), 0.02),
        "od_w_in": _normal(ks[7], (N_ODD, D, ODD_IN), D ** -0.5),
        "od_w_out": _normal(ks[8], (N_ODD, ODD_OUT, D), DN_BETA * ODD_OUT ** -0.5),
        "mla_q_norm": 1.0 + _normal(ks[9], (N_ODD, MLA_Q_RANK), 0.02),
        "mla_w_q_up": _normal(ks[10], (N_ODD, MLA_Q_RANK, MLA_HEADS * (MLA_NOPE + MLA_ROPE)),
                              MLA_Q_RANK ** -0.5),
        "mla_kv_norm": 1.0 + _normal(ks[11], (N_ODD, MLA_KV_RANK), 0.02),
        "mla_w_kv_up": _normal(ks[12], (N_ODD, MLA_KV_RANK, MLA_HEADS * (MLA_NOPE + MLA_V)),
                               MLA_KV_RANK ** -0.5),
        "gqa_q_norm": 1.0 + _normal(ks[13], (N_ODD, GQA_DIM), 0.02),
        "gqa_k_norm": 1.0 + _normal(ks[14], (N_ODD, GQA_DIM), 0.02),
        "ln1_g": 1.0 + _normal(ks[15], (DEPTH, D), 0.02),
        "ln1_b": _normal(ks[16], (DEPTH, D), 0.02),
        "ln2_g": 1.0 + _normal(ks[17], (DEPTH, D), 0.02),
        "ln2_b": _normal(ks[18], (DEPTH, D), 0.02),
        "moe_w_group": _normal(ks[19], (DEPTH, D, N_GROUPS), D ** -0.5),
        "moe_b_group": _normal(ks[20], (DEPTH, N_GROUPS), 0.01),
        "moe_w_expert": _normal(ks[21], (DEPTH, D, N_EXPERTS), D ** -0.5),
        "moe_b_expert": _normal(ks[22], (DEPTH, N_EXPERTS), 0.01),
        "moe_w1": _normal(ks[23], (DEPTH, N_EXPERTS, D, D_EXPERT), D ** -0.5),
        "moe_w3": _normal(ks[24], (DEPTH, N_EXPERTS, D, D_EXPERT), D ** -0.5),
        "moe_w2": _normal(ks[25], (DEPTH, N_EXPERTS, D_EXPERT, D), DN_BETA * D_EXPERT ** -0.5),
    }


def reference(x_prompt, x_sample, ev_w_in, ev_w_out, diff_lambda, diff_subln, na_rpb,
              od_w_in, od_w_out, mla_q_norm, mla_w_q_up, mla_kv_norm, mla_w_kv_up,
              gqa_q_norm, gqa_k_norm, ln1_g, ln1_b, ln2_g, ln2_b,
              moe_w_group, moe_b_group, moe_w_expert, moe_b_expert, moe_w1, moe_w3, moe_w2):
    y_prompt = _trunk(x_prompt, ev_w_in, ev_w_out, diff_lambda, diff_subln, na_rpb,
                      od_w_in, od_w_out, mla_q_norm, mla_w_q_up, mla_kv_norm, mla_w_kv_up,
                      gqa_q_norm, gqa_k_norm, ln1_g, ln1_b, ln2_g, ln2_b,
                      moe_w_group, moe_b_group, moe_w_expert, moe_b_expert, moe_w1, moe_w3, moe_w2)
    y_sample = _trunk(x_sample, ev_w_in, ev_w_out, diff_lambda, diff_subln, na_rpb,
                      od_w_in, od_w_out, mla_q_norm, mla_w_q_up, mla_kv_norm, mla_w_kv_up,
                      gqa_q_norm, gqa_k_norm, ln1_g, ln1_b, ln2_g, ln2_b,
                      moe_w_group, moe_b_group, moe_w_expert, moe_b_expert, moe_w1, moe_w3, moe_w2)
    return (y_prompt, y_sample)
```

```python
import math
from contextlib import ExitStack

import numpy as np
import concourse.bass as bass
import concourse.mybir as mybir
from concourse.bass_utils import run_bass_kernel_spmd

F32 = mybir.dt.float32
BF16 = mybir.dt.bfloat16
I32 = mybir.dt.int32
AF = mybir.ActivationFunctionType
ALU = mybir.AluOpType
AX = mybir.AxisListType

D = 2048
KC = 16
GRID_W = 64
DEPTH = 2
DN_ALPHA = (2 * DEPTH) ** 0.25
LN_EPS = 1e-5
RMS_EPS = 1e-6
NEG = -30000.0
SEM_LIMIT = 30000


_UID = [0]


def U(name):
    _UID[0] += 1
    return f"{name}_u{_UID[0]}"


class Buf:
    __slots__ = ("w", "r")

    def __init__(self):
        self.w = None
        self.r = {}


class _Eng:
    def __init__(self, s, name, eng):
        self.s = s
        self.name = name
        self.eng = eng
        self.sem = s.nc.alloc_semaphore(f"es_{name}_0")
        self.nsem = 1
        self.count = 0
        self.seen = {}

    def bump(self):
        if self.count >= SEM_LIMIT:
            self.sem = self.s.nc.alloc_semaphore(f"es_{self.name}_{self.nsem}")
            self.nsem += 1
            self.count = 0
        self.count += 1
        return (self.sem, self.count, None)


class _DmaSem:
    def __init__(self, s, i):
        self.s = s
        self.i = i
        self.gen = 0
        self.sem = s.nc.alloc_semaphore(f"ds_{i}_0")
        self.count = 0
        self.final = {}

    def bump(self):
        if self.count + 16 > SEM_LIMIT:
            self.final[id(self.sem)] = self.count
            self.gen += 1
            self.sem = self.s.nc.alloc_semaphore(f"ds_{self.i}_{self.gen}")
            self.count = 0
        self.count += 16
        return (self.sem, self.count, self)


class Sched:
    def __init__(self, nc, n_dma_sems=16):
        self.nc = nc
        self.E = {
            "pe": _Eng(self, "pe", nc.tensor),
            "act": _Eng(self, "act", nc.scalar),
            "dve": _Eng(self, "dve", nc.vector),
            "pool": _Eng(self, "pool", nc.gpsimd),
            "sp": _Eng(self, "sp", nc.sync),
        }
        self.dsems = [_DmaSem(self, i) for i in range(n_dma_sems)]
        self.dnext = 0
        self.swsems = [_DmaSem(self, 100 + i) for i in range(8)]
        self.swnext = 0
        self.n_ins = 0
        self.n_wait = 0

    def _wait(self, E, tok):
        sem, val, ds = tok
        if ds is not None:
            val = max(val, ds.count if ds.sem is sem else ds.final[id(sem)])
        key = id(sem)
        if E.seen.get(key, 0) >= val:
            return
        E.eng.wait_ge(sem, val)
        E.seen[key] = val
        self.n_wait += 1

    def _deps(self, E, reads, writes, skip_same):
        for b in reads:
            t = b.w
            if t is not None and not (skip_same and t[2] is None and t[0] is E.sem):
                self._wait(E, t)
        for b in writes:
            t = b.w
            if t is not None and not (skip_same and t[2] is None and t[0] is E.sem):
                self._wait(E, t)
            for t in b.r.values():
                if not (skip_same and t[2] is None and t[0] is E.sem):
                    self._wait(E, t)

    def _mark(self, tok, reads, writes):
        k = id(tok[0])
        for b in reads:
            b.r[k] = tok
        for b in writes:
            b.w = tok
            b.r = {}

    def op(self, eng, fn, reads=(), writes=()):
        E = self.E[eng]
        self._deps(E, reads, writes, eng == "pe")
        ins = fn(E.eng)
        tok = E.bump()
        ins.then_inc(tok[0], 1)
        self._mark(tok, reads, writes)
        self.n_ins += 1
        return ins

    def dma(self, q, out, in_, reads=(), writes=(), **kw):
        E = self.E[q]
        self._deps(E, reads, writes, False)
        if q == "pool":
            ds = self.swsems[self.swnext]
            self.swnext = (self.swnext + 1) % len(self.swsems)
            if ds.count > 0:
                self._wait(E, (ds.sem, ds.count, ds))
        else:
            ds = self.dsems[self.dnext]
            self.dnext = (self.dnext + 1) % len(self.dsems)
        ins = E.eng.dma_start(out=out, in_=in_, **kw)
        tok = ds.bump()
        ins.then_inc(tok[0], 16)
        self._mark(tok, reads, writes)
        self.n_ins += 1
        return ins

    def idma(self, out, out_off, in_, in_off, reads=(), writes=(), **kw):
        E = self.E["pool"]
        self._deps(E, reads, writes, False)
        ds = self.swsems[self.swnext]
        self.swnext = (self.swnext + 1) % len(self.swsems)
        if ds.count > 0:
            self._wait(E, (ds.sem, ds.count, ds))
        ins = E.eng.indirect_dma_start(out, out_off, in_, in_off, **kw)
        tok = ds.bump()
        ins.then_inc(tok[0], 16)
        self._mark(tok, reads, writes)
        self.n_ins += 1
        return ins

    def barrier(self):
        for E in self.E.values():
            for F in self.E.values():
                if F is not E and F.count > 0:
                    self._wait(E, (F.sem, F.count, None))
            for ds in self.dsems + self.swsems:
                if ds.count > 0:
                    self._wait(E, (ds.sem, ds.count, ds))


def bcast_rows(ap2d_row, nparts):
    t = ap2d_row
    return bass.AP(t.tensor, t.offset, [[0, nparts]] + [list(x) for x in t.ap[1:]])


class KB:
    def __init__(self, nc):
        self.nc = nc
        self.S = Sched(nc)
        self.pq = [nc.alloc_psum_tensor(f"pq{i}", [128, 1024], F32) for i in range(4)]
        self.pb = [Buf() for _ in range(8)]
        self.reg = {}
        self.db = {}
        kb = self

        class _Lazy(dict):
            def __missing__(d, name):
                shp = kb.reg[name]
                t = kb.nc.dram_tensor(name, list(shp), F32, kind="ExternalInput")
                d[name] = t.ap()
                kb.db[name] = Buf()
                return d[name]
        self.dr = _Lazy()

    def bank(self, i):
        return self.pq[i // 2][:, (i % 2) * 512:(i % 2) * 512 + 512], self.pb[i]

    def dram(self, name, shape, dt, kind="Internal"):
        t = self.nc.dram_tensor(name, list(shape), dt, kind=kind)
        self.dr[name] = t.ap()
        self.db[name] = Buf()
        return self.dr[name]


def load_consts(kb, es):
    nc, S = kb.nc, kb.S
    c = {}
    cb = Buf()

    def sb(name, shape, dt):
        return es.enter_context(nc.sbuf_tensor(U(name), shape, dt))

    c["ident"] = sb("k_ident", [128, 128], F32)
    c["ones_f"] = sb("k_onesf", [128, 128], F32)
    c["ones_b"] = sb("k_onesb", [128, 128], BF16)
    c["PA"] = sb("k_PA", [128, 128], BF16)
    c["PG"] = sb("k_PG", [128, 128], BF16)
    c["PM"] = sb("k_PM", [64, 64], BF16)
    c["U"] = sb("k_U", [128, 128], F32)
    c["J"] = sb("k_J", [64, 64], F32)
    c["mneg"] = sb("k_mneg", [128, 64], F32)
    S.dma("sp", c["ident"][:], kb.dr["c_ident"][:, :], writes=[cb])
    S.dma("sp", c["U"][:], kb.dr["c_U"][:, :], writes=[cb])
    S.dma("sp", c["J"][:], kb.dr["c_J"][:, :], writes=[cb])
    S.dma("sp", c["mneg"][:], kb.dr["c_mneg"][:, :], writes=[cb])
    S.dma("pool", c["PA"][:], kb.dr["c_PA"][:, :], writes=[cb])
    S.dma("pool", c["PG"][:], kb.dr["c_PG"][:, :], writes=[cb])
    S.dma("pool", c["PM"][:], kb.dr["c_PM"][:, :], writes=[cb])
    S.op("dve", lambda e: e.memset(c["ones_f"][:], 1.0), writes=[cb])
    S.op("dve", lambda e: e.memset(c["ones_b"][:], 1.0), writes=[cb])
    c["buf"] = cb
    return c


def load_w_bf16(kb, dst, dbuf, src2d, ksplit=4):
    K = src2d.shape[0]
    kc = K // 128
    v = src2d.rearrange("(kc p) n -> p kc n", p=128)
    step = max(1, kc // ksplit)
    for k0 in range(0, kc, step):
        kb.S.dma("pool", dst[:, k0:k0 + step, :], v[:, k0:k0 + step, :], writes=[dbuf])


def rope_evac(kb, c, A, bA, Bk, bB, Pm, np_, ctab, stab, btab, qa, bqa, t1, bt1, t2, bt2, out, bout):
    S = kb.S
    import os
    R_ = int(os.environ.get("ROPE_STEPS", "5"))
    S.op("act", lambda e: e.activation(out=qa[:np_, :], in_=A[:np_, :], func=AF.Copy), reads=[bA], writes=[bqa])
    if R_ < 2:
        return
    S.op("pe", lambda e: e.matmul(Bk[:np_, :], Pm[:np_, :np_], qa[:np_, :], start=True, stop=True),
         reads=[bqa, c["buf"]], writes=[bB])
    if R_ < 3:
        return
    S.op("act", lambda e: e.activation(out=t1[:np_, :], in_=A[:np_, :], func=AF.Copy), reads=[bA], writes=[bt1])
    S.op("dve", lambda e: e.tensor_tensor(out=t1[:np_, :], in0=t1[:np_, :], in1=ctab[:np_, :], op=ALU.mult),
         reads=[btab, bt1], writes=[bt1])
    if R_ < 4:
        return
    S.op("dve", lambda e: e.tensor_tensor(out=t2[:np_, :], in0=Bk[:np_, :], in1=stab[:np_, :], op=ALU.mult),
         reads=[bB, btab], writes=[bt2])
    if R_ < 5:
        return
    S.op("dve", lambda e: e.tensor_tensor(out=out[:np_, :], in0=t1[:np_, :], in1=t2[:np_, :], op=ALU.add),
         reads=[bt1, bt2], writes=[bout])


def s1_l0(kb, c, job):
    nc, S = kb.nc, kb.S
    n = job["name"]
    Sn = job["S"]
    NT = Sn // 512
    xT = kb.dr[f"xT_{n}"]
    w_in = kb.dr["ev_w_in"]
    S.barrier()
    with ExitStack() as es:
        def sb(name, shape, dt):
            return es.enter_context(nc.sbuf_tensor(U(name), shape, dt))
        wg = [sb(f"s1wg{i}", [128, 16, 1024], BF16) for i in range(2)]
        bwg = [Buf(), Buf()]
        xt = [sb(f"s1xt{i}", [128, 16, 512], BF16) for i in range(2)]
        bxt = [Buf(), Buf()]
        ct = [sb(f"s1ct{i}", [128, 512], F32) for i in range(2)]
        st = [sb(f"s1st{i}", [128, 512], F32) for i in range(2)]
        btab = [Buf(), Buf()]
        qa = sb("s1qa", [128, 512], BF16); bqa = Buf()
        t1 = sb("s1t1", [128, 512], F32); bt1 = Buf()
        t2 = sb("s1t2", [128, 512], F32); bt2 = Buf()
        og = [sb(f"s1og{i}", [128, 512], BF16) for i in range(3)]
        bog = [Buf() for _ in range(3)]
        vs = [sb(f"s1vs{i}", [128, 1024], BF16) for i in range(2)]
        bvs = [Buf(), Buf()]
        gnames = ["qTa", "kTa", "va", "qTb", "kTb", "vb"]
        it = 0
        io = 0
        iv = 0
        ib = 0
        groups = job.get("groups", list(range(6)))
        load_w_bf16(kb, wg[0], bwg[0], w_in[:, groups[0] * 1024:(groups[0] + 1) * 1024])
        for gi, g in enumerate(groups):
            if gi + 1 < len(groups):
                g1 = groups[gi + 1]
                load_w_bf16(kb, wg[(gi + 1) % 2], bwg[(gi + 1) % 2], w_in[:, g1 * 1024:(g1 + 1) * 1024])
            W = wg[gi % 2]
            bW = bwg[gi % 2]
            dst = kb.dr[f"{gnames[g]}_{n}"]
            bdst = kb.db[f"{gnames[g]}_{n}"]
            for t in range(NT):
                X = xt[it % 2]
                bX = bxt[it % 2]
                src = xT[:, t * 512:(t + 1) * 512].rearrange("(kc p) n -> p kc n", p=128)
                for k0 in range(0, 16, 8):
                    S.dma("pool", X[:, k0:k0 + 8, :], src[:, k0:k0 + 8, :], writes=[bX])
                if g in (0, 1):
                    C_ = ct[it % 2]
                    S_ = st[it % 2]
                    bT = btab[it % 2]
                    S.dma("sp", C_[:], kb.dr["ropeA_c"][:, t * 512:(t + 1) * 512], writes=[bT])
                    S.dma("sp", S_[:], kb.dr["ropeA_s"][:, t * 512:(t + 1) * 512], writes=[bT])
                it += 1
                if g in (0, 1, 3, 4):
                    for h in range(job.get("heads", 8)):
                        A, bA = kb.bank(ib % 6)
                        ib += 1
                        for kc in range(16):
                            S.op("pe", lambda e: e.matmul(A, W[:, kc, h * 128:(h + 1) * 128], X[:, kc, :],
                                                          start=(kc == 0), stop=(kc == 15)),
                                 reads=[bW, bX], writes=[bA])
                        O = og[io % 3]
                        bO = bog[io % 3]
                        io += 1
                        if g in (0, 1):
                            Bk, bB = kb.bank(6 + (ib % 2))
                            rope_evac(kb, c, A, bA, Bk, bB, c["PA"], 128, C_, S_, bT, qa, bqa, t1, bt1, t2, bt2, O, bO)
                        else:
                            S.op("act", lambda e: e.activation(out=O[:], in_=A, func=AF.Copy), reads=[bA], writes=[bO])
                        if not job.get("nostore"):
                            S.dma("sp", dst[h, :, t * 512:(t + 1) * 512], O[:], reads=[bO], writes=[bdst])
                else:
                    for sub in range(4):
                        V = vs[iv % 2]
                        bV = bvs[iv % 2]
                        iv += 1
                        for half in range(2):
                            A, bA = kb.bank(ib % 6)
                            ib += 1
                            for kc in range(16):
                                S.op("pe", lambda e: e.matmul(A, X[:, kc, sub * 128:(sub + 1) * 128],
                                                              W[:, kc, half * 512:(half + 1) * 512],
                                                              start=(kc == 0), stop=(kc == 15)),
                                     reads=[bW, bX], writes=[bA])
                            if half == 0:
                                S.op("act", lambda e: e.activation(out=V[:, 0:512], in_=A, func=AF.Copy),
                                     reads=[bA], writes=[bV])
                            else:
                                S.op("dve", lambda e: e.tensor_copy(out=V[:, 512:1024], in_=A),
                                     reads=[bA], writes=[bV])
                        r0 = t * 512 + sub * 128
                        S.dma("sp", dst[r0:r0 + 128, :], V[:], reads=[bV], writes=[bdst])
    S.barrier()


class AttnRes:
    def __init__(self, kb, es, tag):
        nc = kb.nc
        self.pT = [es.enter_context(nc.sbuf_tensor(U(f"{tag}pT{i}"), [128, 1024], BF16)) for i in range(3)]
        self.bpT = [Buf() for _ in range(3)]
        self.accs = es.enter_context(nc.sbuf_tensor(U(f"{tag}accs"), [128, 1024], F32))
        self.baccs = Buf()
        self.rs = es.enter_context(nc.sbuf_tensor(U(f"{tag}rs"), [128, 512], F32))
        self.brs = Buf()
        self.i = 0


def attn_tile(kb, c, R, Sn, terms, v, bv, scale, om, bom):
    S = kb.S
    OT, bOT = kb.bank(6)
    MS, bMS = kb.bank(7)
    nb = Sn // 256
    for b2 in range(nb):
        i = R.i % 3
        R.i += 1
        ps = kb.pq[i]
        bps = [kb.pb[2 * i], kb.pb[2 * i + 1]]
        for j in range(2):
            kc = 2 * b2 + j
            for ti, (kfn, q, tb) in enumerate(terms):
                S.op("pe", lambda e: e.matmul(ps[:, j * 512:(j + 1) * 512], kfn(kc), q,
                                              start=(ti == 0), stop=(ti == len(terms) - 1)),
                     reads=tb, writes=[bps[j]])
        pT = R.pT[i]
        bpT = R.bpT[i]
        S.op("act", lambda e: e.activation(out=pT[:], in_=ps[:, :], func=AF.Exp, scale=scale),
             reads=bps, writes=[bpT])
        for j in range(2):
            kc = 2 * b2 + j
            S.op("pe", lambda e: e.matmul(OT, v[:, kc, :], pT[:, j * 512:(j + 1) * 512],
                                          start=(kc == 0), stop=(kc == Sn // 128 - 1)),
                 reads=[bv, bpT], writes=[bOT])
        if b2 == 0:
            S.op("dve", lambda e: e.tensor_copy(out=R.accs[:], in_=pT[:]), reads=[bpT], writes=[R.baccs])
        else:
            S.op("dve", lambda e: e.tensor_tensor(out=R.accs[:], in0=R.accs[:], in1=pT[:], op=ALU.add),
                 reads=[bpT, R.baccs], writes=[R.baccs])
    for j in range(2):
        S.op("pe", lambda e: e.matmul(MS, c["ones_f"][:], R.accs[:, j * 512:(j + 1) * 512],
                                      start=(j == 0), stop=(j == 1)),
             reads=[R.baccs, c["buf"]], writes=[bMS])
    S.op("dve", lambda e: e.reciprocal(out=R.rs[:], in_=MS), reads=[bMS], writes=[R.brs])
    S.op("dve", lambda e: e.tensor_tensor(out=om[:], in0=OT, in1=R.rs[:], op=ALU.mult),
         reads=[bOT, R.brs], writes=[bom])


def load_kv(kb, kT, bkT, ksrc, v, bv, vsrc_cols, Sn):
    S = kb.S
    nsp = 4 if Sn >= 2048 else 1
    w = Sn // nsp
    for i in range(nsp):
        S.dma("sp", kT[:, i * w:(i + 1) * w], ksrc[:, i * w:(i + 1) * w], writes=[bkT])
    vv = vsrc_cols.rearrange("(c p) d -> p c d", p=128)
    nc_ = Sn // 128
    wv = min(8, nc_)
    for i in range(0, nc_, wv):
        S.dma("sp", v[:, i:i + wv, :], vv[:, i:i + wv, :], writes=[bv])


def s2_diff(kb, c, job):
    nc, S = kb.nc, kb.S
    n = job["name"]
    Sn = job["S"]
    NT = Sn // 512
    qTa, kTa, va, oT = (kb.dr[f"{x}_{n}"] for x in ("qTa", "kTa", "va", "oT"))
    boT = kb.db[f"oT_{n}"]
    lam_init = 0.8 - 0.6 * math.exp(-0.3 * 0)
    S.barrier()
    with ExitStack() as es:
        def sb(name, shape, dt):
            return es.enter_context(nc.sbuf_tensor(U(name), shape, dt))
        R = AttnRes(kb, es, "s2")
        kT = sb("s2kT", [128, Sn], BF16); bkT = Buf()
        v = sb("s2v", [128, Sn // 128, 128], BF16); bv = Buf()
        qT = [sb(f"s2q{i}", [128, 512], BF16) for i in range(2)]
        bq = [Buf(), Buf()]
        om = [sb(f"s2om{i}", [128, 512], F32) for i in range(2)]
        bom = [Buf(), Buf()]
        o = sb("s2o", [128, 512], F32); bo = Buf()
        sq = sb("s2sq", [128, 512], F32); bsq = Buf()
        rstd = sb("s2rstd", [128, 512], F32); brstd = Buf()
        ob = [sb(f"s2ob{i}", [128, 512], BF16) for i in range(2)]
        bob = [Buf(), Buf()]
        lv = sb("s2lv", [128, 4, 64], F32); blv = Buf()
        pr = sb("s2pr", [128, 2, 64], F32); bpr = Buf()
        sm = sb("s2sm", [128, 2], F32); bsm = Buf()
        ex = sb("s2ex", [128, 2], F32); bex = Buf()
        nlam = sb("s2nlam", [128, 1], F32); bnl = Buf()
        gcol = sb("s2gcol", [128, 1], F32); bg = Buf()
        dl = kb.dr["diff_lambda"]
        S.dma("sp", lv[:].rearrange("p a b -> p (a b)"),
              bcast_rows(dl.rearrange("a b -> (a b)")[None, :], 128) if False else
              bass.AP(dl.tensor, dl.offset, [[0, 128], [1, 256]]), writes=[blv])
        S.op("dve", lambda e: e.tensor_tensor(out=pr[:, 0, :], in0=lv[:, 0, :], in1=lv[:, 1, :], op=ALU.mult),
             reads=[blv], writes=[bpr])
        S.op("dve", lambda e: e.tensor_tensor(out=pr[:, 1, :], in0=lv[:, 2, :], in1=lv[:, 3, :], op=ALU.mult),
             reads=[blv], writes=[bpr])
        S.op("dve", lambda e: e.tensor_reduce(out=sm[:], in_=pr[:], axis=AX.X, op=ALU.add), reads=[bpr], writes=[bsm])
        S.op("act", lambda e: e.activation(out=ex[:], in_=sm[:], func=AF.Exp), reads=[bsm], writes=[bex])
        S.op("dve", lambda e: e.tensor_tensor(out=nlam[:], in0=ex[:, 1:2], in1=ex[:, 0:1], op=ALU.subtract),
             reads=[bex], writes=[bnl])
        S.op("dve", lambda e: e.tensor_scalar(out=nlam[:], in0=nlam[:], scalar1=-lam_init, scalar2=None, op0=ALU.add),
             reads=[bnl], writes=[bnl])
        g = kb.dr["diff_subln"]
        S.dma("sp", gcol[:], bass.AP(g.tensor, g.offset, [[1, 128], [1, 1]]), writes=[bg])
        S.op("dve", lambda e: e.tensor_scalar(out=gcol[:], in0=gcol[:], scalar1=(1.0 - lam_init), scalar2=None,
                                              op0=ALU.mult), reads=[bg], writes=[bg])
        iq = 0
        MS, bMS = kb.bank(7)
        for h in range(8):
            load_kv(kb, kT, bkT, kTa[h], v, bv, va[:, h * 128:(h + 1) * 128], Sn)
            for t in range(NT):
                Q = qT[iq % 2]
                bQ = bq[iq % 2]
                S.dma("sp", Q[:], qTa[h, :, t * 512:(t + 1) * 512], writes=[bQ])
                for m in range(2):
                    lo = 64 * m
                    terms = [(lambda kc, lo=lo: kT[lo:lo + 64, kc * 128:(kc + 1) * 128], Q[lo:lo + 64, :], [bkT, bQ])]
                    attn_tile(kb, c, R, Sn, terms, v, bv, 0.125, om[m], bom[m])
                S.op("dve", lambda e: e.scalar_tensor_tensor(out=o[:], in0=om[1][:], scalar=nlam[:, 0:1], in1=om[0][:],
                                                             op0=ALU.mult, op1=ALU.add),
                     reads=[bom[0], bom[1], bnl], writes=[bo])
                S.op("act", lambda e: e.activation(out=sq[:], in_=o[:], func=AF.Square), reads=[bo], writes=[bsq])
                S.op("pe", lambda e: e.matmul(MS, c["ones_f"][:], sq[:], start=True, stop=True),
                     reads=[bsq, c["buf"]], writes=[bMS])
                S.op("dve", lambda e: e.tensor_scalar(out=rstd[:], in0=MS, scalar1=1.0 / 128, scalar2=1e-5,
                                                      op0=ALU.mult, op1=ALU.add), reads=[bMS], writes=[brstd])
                S.op("act", lambda e: e.activation(out=rstd[:], in_=rstd[:], func=AF.Sqrt), reads=[brstd], writes=[brstd])
                S.op("dve", lambda e: e.reciprocal(out=rstd[:], in_=rstd[:]), reads=[brstd], writes=[brstd])
                S.op("dve", lambda e: e.tensor_tensor(out=o[:], in0=o[:], in1=rstd[:], op=ALU.mult),
                     reads=[bo, brstd], writes=[bo])
                OB = ob[iq % 2]
                bOB = bob[iq % 2]
                S.op("dve", lambda e: e.tensor_scalar(out=OB[:], in0=o[:], scalar1=gcol[:, 0:1], scalar2=None,
                                                      op0=ALU.mult), reads=[bo, bg], writes=[bOB])
                S.dma("sp", oT[h * 128:(h + 1) * 128, t * 512:(t + 1) * 512], OB[:], reads=[bOB], writes=[boT])
                iq += 1
    S.barrier()


def host_consts(smax):
    pos = np.arange(smax, dtype=np.float32)
    out = {}
    inv = (500000.0 ** (-np.arange(0, 16, 2, dtype=np.float32) / 16)).astype(np.float32)
    ang = pos[None, :] * inv[:, None]
    cA = np.ones((128, smax), np.float32)
    sA = np.zeros((128, smax), np.float32)
    PA = np.zeros((128, 128), np.float32)
    for m in range(2):
        for j in range(8):
            a, b = 64 * m + j, 64 * m + j + 8
            cA[a] = np.cos(ang[j]); cA[b] = np.cos(ang[j])
            sA[a] = -np.sin(ang[j]); sA[b] = np.sin(ang[j])
            PA[a, b] = 1.0; PA[b, a] = 1.0
    out["ropeA_c"], out["ropeA_s"], out["c_PA"] = cA, sA, PA
    inv = (10000.0 ** (-np.arange(0, 64, 2, dtype=np.float32) / 64)).astype(np.float32)
    ang = pos[None, :] * inv[:, None]
    cM = np.concatenate([np.cos(ang), np.cos(ang)], 0).astype(np.float32)
    sM = np.concatenate([-np.sin(ang), np.sin(ang)], 0).astype(np.float32)
    PM = np.zeros((64, 64), np.float32)
    for j in range(32):
        PM[j, j + 32] = 1.0; PM[j + 32, j] = 1.0
    out["ropeM_c"], out["ropeM_s"], out["c_PM"] = cM, sM, PM
    angr = (pos // GRID_W)[None, :] * inv[:, None]
    angc = (pos % GRID_W)[None, :] * inv[:, None]
    cG = np.concatenate([np.cos(angr), np.cos(angr), np.cos(angc), np.cos(angc)], 0).astype(np.float32)
    sG = np.concatenate([-np.sin(angr), np.sin(angr), -np.sin(angc), np.sin(angc)], 0).astype(np.float32)
    PG = np.zeros((128, 128), np.float32)
    PG[:64, :64] = PM
    PG[64:, 64:] = PM
    out["ropeG_c"], out["ropeG_s"], out["c_PG"] = cG, sG, PG
    out["c_ident"] = np.eye(128, dtype=np.float32)
    out["c_U"] = np.triu(np.ones((128, 128), np.float32), 1)
    out["c_J"] = np.eye(64, dtype=np.float32)[::-1].copy()
    cols = np.arange(64)
    cs = np.clip(cols - 8, 0, 48)
    m = np.full((64, 64), NEG, np.float32)
    for cq in range(64):
        m[cs[cq]:cs[cq] + 16, cq] = 0.0
    out["c_mneg"] = np.concatenate([m, m], 0)
    return out


WEIGHT_SHAPES = {
    "ev_w_in": [2048, 6144], "ev_w_out": [2048, 2048], "diff_lambda": [4, 64], "diff_subln": [1, 128],
    "na_rpb": [8, 15, 31], "od_w_in": [2048, 2368], "od_w_out": [2048, 2048], "mla_q_norm": [1, 512],
    "mla_w_q_up": [512, 1536], "mla_kv_norm": [1, 256], "mla_w_kv_up": [256, 2048], "gqa_q_norm": [1, 128],
    "gqa_k_norm": [1, 128], "ln1_g": [2, 2048], "ln1_b": [2, 2048], "ln2_g": [2, 2048], "ln2_b": [2, 2048],
    "moe_w_group": [2, 2048, 4], "moe_b_group": [2, 4], "moe_w_expert": [2, 2048, 32], "moe_b_expert": [2, 32],
    "moe_w1": [2, 32, 2048, 512], "moe_w3": [2, 32, 2048, 512], "moe_w2": [2, 32, 512, 2048],
}


def build_program(jobs, stages=None, debug=False, wnames=None):
    nc = bass.Bass("TRN2", target_bir_lowering=False)
    kb = KB(nc)
    smax = max(j["S"] for j in jobs)
    skind = "ExternalOutput" if debug else "Internal"
    for name, shp in WEIGHT_SHAPES.items():
        kb.reg[name] = shp
    hc = host_consts(128)
    for name, arr in hc.items():
        shp = list(arr.shape)
        if name.startswith("rope"):
            shp[1] = smax
        kb.reg[name] = shp
    kb.dram("rpbpad", [8, 15, 160], F32)
    for j in jobs:
        n, Sn = j["name"], j["S"]
        Cs = j["cap"] + 128
        kb.reg[f"x_{n}"] = [Sn, D]
        kb.reg[f"xT_{n}"] = [D, Sn]
        kb.reg[f"ecs_{n}"] = [128, 32]
        kb.dram(f"y_{n}", [Sn, D], F32, kind="ExternalOutput")
        for t in ("qTa", "kTa", "qTb", "kTb"):
            kb.dram(f"{t}_{n}", [8, 128, Sn], BF16, kind=skind)
        kb.dram(f"va_{n}", [Sn, 1024], BF16, kind=skind)
        kb.dram(f"vb_{n}", [Sn, 1024], BF16, kind=skind)
        kb.dram(f"oT_{n}", [D, Sn], BF16, kind=skind)
        kb.dram(f"xmid_{n}", [Sn, D], F32, kind=skind)
        kb.dram(f"xbuf_{n}", [32 * Cs, D], BF16)
        kb.dram(f"ybuf_{n}", [32 * Cs, D], BF16)
        kb.dram(f"x1_{n}", [Sn, D], F32, kind=skind)
        kb.dram(f"x1T_{n}", [D, Sn], BF16, kind=skind)
        for t in ("qnT", "knT", "gqT"):
            kb.dram(f"{t}_{n}", [8, 128, Sn], BF16, kind=skind)
        kb.dram(f"qpeT_{n}", [8, 64, Sn], BF16, kind=skind)
        kb.dram(f"kpeT_{n}", [64, Sn], BF16, kind=skind)
        kb.dram(f"gkT_{n}", [2, 128, Sn], BF16, kind=skind)
        kb.dram(f"vm_{n}", [Sn, 1024], BF16, kind=skind)
        kb.dram(f"gv_{n}", [Sn, 256], BF16, kind=skind)
    def on(x):
        return stages is None or x in stages
    with ExitStack() as es:
        c = load_consts(kb, es)
        for j in jobs:
            n, Sn = j["name"], j["S"]
            P = {"idx": es.enter_context(nc.sbuf_tensor(f"P_idx_{n}", [128, Sn // 128, 2], I32)),
                 "gate": es.enter_context(nc.sbuf_tensor(f"P_gate_{n}", [128, Sn // 128, 2], F32)),
                 "bidx": Buf(), "bgate": Buf()}
            if on("s1"):
                s1_l0(kb, c, j)
            if on("s2"):
                s2_diff(kb, c, j)
            if on("s3"):
                s3_na(kb, c, j)
            if on("s4"):
                s45_post(kb, c, j, 0, P)
            if on("s6"):
                s6_experts(kb, c, j, 0)
            if on("s7"):
                s7_combine(kb, c, j, 0, P)
            if on("l1"):
                s1_l1(kb, c, j)
                s2_mla(kb, c, j)
                s3_gqa(kb, c, j)
                s45_post(kb, c, j, 1, P)
                s6_experts(kb, c, j, 1)
                s7_combine(kb, c, j, 1, P)
        kb.S.barrier()
    return nc, kb


def s3_na(kb, c, job):
    nc, S = kb.nc, kb.S
    n = job["name"]
    Sn = job["S"]
    rows = Sn // GRID_W
    NT = Sn // 512
    qTb, kTb, vb, oT = (kb.dr[f"{x}_{n}"] for x in ("qTb", "kTb", "vb", "oT"))
    boT = kb.db[f"oT_{n}"]
    rpb = kb.dr["na_rpb"]
    pad = kb.dr["rpbpad"]
    bpad = kb.db["rpbpad"]
    scale = 128 ** -0.5

    def rs(r):
        return min(max(r - 4, 0), rows - 8)

    S.barrier()
    with ExitStack() as es:
        def sb(name, shape, dt):
            return es.enter_context(nc.sbuf_tensor(U(name), shape, dt))
        z = sb("s3z", [120, 160], F32); bz = Buf()
        S.op("dve", lambda e: e.memset(z[:], 0.0), writes=[bz])
        S.dma("sp", z[:, 64:95], rpb.rearrange("h a w -> (h a) w"), writes=[bz])
        S.dma("sp", pad.rearrange("h a w -> (h a) w"), z[:], reads=[bz], writes=[bpad])
        kT = sb("s3kT", [128, Sn], BF16); bkT = Buf()
        v = sb("s3v", [128, Sn // 128, 128], BF16); bv = Buf()
        tpp = sb("s3tpp", [64, 15, 128], F32); btpp = Buf()
        Tm = sb("s3Tm", [128, 15, 64], F32); bTm = Buf()
        NPAT = 8
        Bt = [sb(f"s3Bt{i}", [128, 5, 128], F32) for i in range(NPAT)]
        bBt = [Buf() for _ in range(NPAT)]
        qT = [sb(f"s3q{i}", [128, 512], BF16) for i in range(2)]
        bq = [Buf(), Buf()]
        sc = [sb(f"s3sc{i}", [128, 640], F32) for i in range(2)]
        bsc = [Buf(), Buf()]
        pT = [sb(f"s3pT{i}", [128, 640], BF16) for i in range(2)]
        bpT = [Buf(), Buf()]
        rsum = sb("s3rs", [128, 512], F32); brs = Buf()
        ob = [sb(f"s3ob{i}", [128, 512], BF16) for i in range(2)]
        bob = [Buf(), Buf()]
        OT, bOT = kb.bank(6)
        MS, bMS = kb.bank(7)
        ib = 0
        iq = 0
        for h in range(8):
            load_kv(kb, kT, bkT, kTb[h], v, bv, vb[:, h * 128:(h + 1) * 128], Sn)
            base = pad[h, 0, 16:17]
            src = bass.AP(base.tensor, base.offset, [[1, 64], [160, 15], [1, 64]])
            S.dma("sp", tpp[:, :, 0:64], src, reads=[bpad], writes=[btpp])
            S.dma("sp", tpp[:, :, 64:128], src, reads=[bpad], writes=[btpp])
            for half in range(2):
                pb_ap = kb.pq[half]
                pbb = [kb.pb[2 * half], kb.pb[2 * half + 1]]
                lo, hi = (0, 8) if half == 0 else (8, 15)
                for dr in range(lo, hi):
                    col = (dr - lo) * 64
                    S.op("pe", lambda e: e.matmul(pb_ap[:, col:col + 64], tpp[:, dr, :], c["J"][:], start=True, stop=True),
                         reads=[btpp, c["buf"]], writes=[pbb[col // 512]])
                for dr in range(lo, hi):
                    col = (dr - lo) * 64
                    S.op("dve", lambda e: e.tensor_tensor(out=Tm[:, dr, :], in0=pb_ap[:, col:col + 64], in1=c["mneg"][:],
                                                          op=ALU.add), reads=[pbb[col // 512], c["buf"]], writes=[bTm])
            pats = {}
            for t in range(NT):
                Q = qT[iq % 2]
                bQ = bq[iq % 2]
                S.dma("sp", Q[:], qTb[h, :, t * 512:(t + 1) * 512], writes=[bQ])
                for bi in range(4):
                    b = t * 4 + bi
                    r0, r1 = 2 * b, 2 * b + 1
                    a0 = rs(r0) // 2
                    a1 = (rs(r1) + 7) // 2
                    chunks = list(range(a0, a1 + 1))
                    key = tuple((kap - r if rs(r) <= kap < rs(r) + 8 else None)
                                for a in chunks for kap in (2 * a, 2 * a + 1) for r in (r0, r1))
                    if key not in pats:
                        pi = len(pats)
                        assert pi < NPAT
                        pats[key] = pi
                        ki = 0
                        for ai, a in enumerate(chunks):
                            for kh in range(2):
                                for rh in range(2):
                                    d = key[ki]
                                    ki += 1
                                    dst = Bt[pi][64 * kh:64 * kh + 64, ai, 64 * rh:64 * rh + 64]
                                    if d is None:
                                        S.op("dve", lambda e: e.memset(dst, NEG), writes=[bBt[pi]])
                                    else:
                                        S.op("dve", lambda e: e.tensor_copy(out=dst, in_=Tm[64 * kh:64 * kh + 64, d + 7, :]),
                                             reads=[bTm], writes=[bBt[pi]])
                    pi = pats[key]
                    nch = len(chunks)
                    i2 = ib % 2
                    ib += 1
                    ps = kb.pq[i2]
                    bps = [kb.pb[2 * i2], kb.pb[2 * i2 + 1]]
                    for ai, a in enumerate(chunks):
                        S.op("pe", lambda e: e.matmul(ps[:, ai * 128:(ai + 1) * 128], kT[:, a * 128:(a + 1) * 128],
                                                      Q[:, bi * 128:(bi + 1) * 128], start=True, stop=True),
                             reads=[bkT, bQ], writes=[bps[ai // 4]])
                    S.op("dve", lambda e: e.scalar_tensor_tensor(
                        out=sc[i2][:, 0:nch * 128], in0=ps[:, 0:nch * 128], scalar=scale,
                        in1=Bt[pi][:, 0:nch, :].rearrange("p a q -> p (a q)"), op0=ALU.mult, op1=ALU.add),
                        reads=bps + [bBt[pi]], writes=[bsc[i2]])
                    S.op("act", lambda e: e.activation(out=pT[i2][:, 0:nch * 128], in_=sc[i2][:, 0:nch * 128], func=AF.Exp),
                         reads=[bsc[i2]], writes=[bpT[i2]])
                    for ai, a in enumerate(chunks):
                        S.op("pe", lambda e: e.matmul(OT[:, bi * 128:(bi + 1) * 128], v[:, a, :], pT[i2][:, ai * 128:(ai + 1) * 128],
                                                      start=(ai == 0), stop=(ai == nch - 1)),
                             reads=[bv, bpT[i2]], writes=[bOT])
                    for ai, a in enumerate(chunks):
                        S.op("pe", lambda e: e.matmul(MS[:, bi * 128:(bi + 1) * 128], c["ones_b"][:], pT[i2][:, ai * 128:(ai + 1) * 128],
                                                      start=(ai == 0), stop=(ai == nch - 1)),
                             reads=[bpT[i2], c["buf"]], writes=[bMS])
                S.op("dve", lambda e: e.reciprocal(out=rsum[:], in_=MS), reads=[bMS], writes=[brs])
                OB = ob[iq % 2]
                bOB = bob[iq % 2]
                S.op("dve", lambda e: e.tensor_tensor(out=OB[:], in0=OT, in1=rsum[:], op=ALU.mult),
                     reads=[bOT, brs], writes=[bOB])
                S.dma("sp", oT[1024 + h * 128:1024 + (h + 1) * 128, t * 512:(t + 1) * 512], OB[:], reads=[bOB], writes=[boT])
                iq += 1
    S.barrier()


def layer_norm_tm(kb, z, bz, st, mv, rstd, bst, gt, bt, bgb):
    S = kb.S
    for i in range(4):
        S.op("dve", lambda e: e.bn_stats(out=st[:, i, :], in_=z[:, i * 512:(i + 1) * 512]), reads=[bz], writes=[bst])
    S.op("dve", lambda e: e.bn_aggr(out=mv[:], in_=st[:]), reads=[bst], writes=[bst])
    S.op("dve", lambda e: e.tensor_scalar(out=rstd[:], in0=mv[:, 1:2], scalar1=LN_EPS, scalar2=None, op0=ALU.add),
         reads=[bst], writes=[bst])
    S.op("act", lambda e: e.activation(out=rstd[:], in_=rstd[:], func=AF.Sqrt), reads=[bst], writes=[bst])
    S.op("dve", lambda e: e.reciprocal(out=rstd[:], in_=rstd[:]), reads=[bst], writes=[bst])
    S.op("dve", lambda e: e.tensor_scalar(out=z[:], in0=z[:], scalar1=mv[:, 0:1], scalar2=rstd[:, 0:1],
                                          op0=ALU.subtract, op1=ALU.mult), reads=[bst, bz], writes=[bz])
    S.op("dve", lambda e: e.tensor_tensor(out=z[:], in0=z[:], in1=gt[:], op=ALU.mult), reads=[bz, bgb], writes=[bz])
    S.op("dve", lambda e: e.tensor_tensor(out=z[:], in0=z[:], in1=bt[:], op=ALU.add), reads=[bz, bgb], writes=[bz])


def load_bcast(kb, dst, bdst, src_row):
    ap = bass.AP(src_row.tensor, src_row.offset, [[0, 128]] + [list(x) for x in src_row.ap])
    kb.S.dma("sp", dst, ap, writes=[bdst])


def s45_post(kb, c, job, layer, P):
    nc, S = kb.nc, kb.S
    n = job["name"]
    Sn = job["S"]
    C = job["cap"]
    Cs = C + 128
    NT = Sn // 512
    oT = kb.dr[f"oT_{n}"]
    boT = kb.db[f"oT_{n}"]
    w_out = kb.dr["ev_w_out" if layer == 0 else "od_w_out"]
    xres = kb.dr[f"x_{n}"] if layer == 0 else kb.dr[f"x1_{n}"]
    bxres = kb.db[f"x_{n}"] if layer == 0 else kb.db[f"x1_{n}"]
    xmid = kb.dr[f"xmid_{n}"]
    bxmid = kb.db[f"xmid_{n}"]
    xbuf = kb.dr[f"xbuf_{n}"]
    bxbuf = kb.db[f"xbuf_{n}"]
    S.barrier()
    with ExitStack() as es:
        def sb(name, shape, dt):
            return es.enter_context(nc.sbuf_tensor(U(name), shape, dt))
        Wo = sb("s4wo", [128, 16, 2048], BF16); bWo = Buf()
        load_w_bf16(kb, Wo, bWo, w_out, ksplit=8)
        gt = sb("s4g", [128, 2048], F32)
        bt = sb("s4b", [128, 2048], F32)
        bgb = Buf()
        load_bcast(kb, gt[:], bgb, kb.dr["ln1_g"][layer])
        load_bcast(kb, bt[:], bgb, kb.dr["ln1_b"][layer])
        Wge = sb("s4wge", [128, 16, 36], F32); bWge = Buf()
        S.dma("sp", Wge[:, :, 0:4], kb.dr["moe_w_group"][layer].rearrange("(kc p) g -> p kc g", p=128), writes=[bWge])
        S.dma("sp", Wge[:, :, 4:36], kb.dr["moe_w_expert"][layer].rearrange("(kc p) g -> p kc g", p=128), writes=[bWge])
        bias = sb("s4bias", [128, 36], F32); bbias = Buf()
        load_bcast(kb, bias[:, 0:4], bbias, kb.dr["moe_b_group"][layer])
        load_bcast(kb, bias[:, 4:36], bbias, kb.dr["moe_b_expert"][layer])
        eC = sb("s4eC", [128, 32], F32); beC = Buf()
        S.dma("sp", eC[:], kb.dr[f"ecs_{n}"][:, :], writes=[beC])
        cnt = sb("s4cnt", [1, 32], F32); bcnt = Buf()
        S.op("dve", lambda e: e.memset(cnt[:], 0.0), writes=[bcnt])
        zt = sb("s4zero", [128, 2048], BF16); bzt = Buf()
        S.op("dve", lambda e: e.memset(zt[:], 0.0), writes=[bzt])
        for r in range(0, 32 * Cs, 128):
            S.dma("sp", xbuf[r:r + 128, :], zt[:], reads=[bzt], writes=[bxbuf])
        ybuf_ = kb.dr[f"ybuf_{n}"]
        for e_ in range(32):
            S.dma("sp", ybuf_[e_ * Cs + C:(e_ + 1) * Cs, :], zt[:], reads=[bzt], writes=[kb.db[f"ybuf_{n}"]])
        ot = [sb(f"s4ot{i}", [128, 16, 512], BF16) for i in range(2)]
        bot = [Buf(), Buf()]
        xt = [sb(f"s4x{i}", [128, 2048], F32) for i in range(2)]
        bxt = [Buf(), Buf()]
        z = [sb(f"s4z{i}", [128, 2048], F32) for i in range(2)]
        bz = [Buf(), Buf()]
        st = sb("s4st", [128, 4, 6], F32)
        mv = sb("s4mv", [128, 2], F32)
        rstd = sb("s4rstd", [128, 1], F32)
        bst = Buf()
        xmT = sb("s4xmT", [128, 16, 128], F32); bxmT = Buf()
        lg = sb("s4lg", [128, 36], F32); blg = Buf()
        r_ = {}
        for nm, shp in (("gmax", [128, 1]), ("gsel", [128, 4]), ("gex", [128, 4]), ("gsum", [128, 1]), ("pg", [128, 1]),
                        ("em", [128, 4, 8]), ("ein", [128, 8]), ("m1", [128, 1]), ("oh1", [128, 8]), ("ein2", [128, 8]),
                        ("m2", [128, 1]), ("oh2", [128, 8]), ("dd", [128, 1]), ("w1", [128, 1]), ("w2", [128, 1]),
                        ("M1", [128, 4, 8]), ("M2", [128, 4, 8]), ("M", [128, 32]), ("rk", [128, 32]), ("pr", [128, 32]),
                        ("sl", [128, 2])):
            r_[nm] = sb(f"s4r_{nm}", shp, F32)
        br = Buf()
        it = 0
        for T in range(NT):
            O = ot[T % 2]
            bO = bot[T % 2]
            src = oT[:, T * 512:(T + 1) * 512].rearrange("(kc p) t -> p kc t", p=128)
            for k0 in range(0, 16, 4):
                S.dma("sp", O[:, k0:k0 + 4, :], src[:, k0:k0 + 4, :], reads=[boT], writes=[bO])
            for sub in range(4):
                t = T * 4 + sub
                X = xt[it % 2]
                bX = bxt[it % 2]
                Z = z[it % 2]
                bZ = bz[it % 2]
                it += 1
                S.dma("sp", X[:], xres[t * 128:(t + 1) * 128, :], reads=[bxres], writes=[bX])
                for cg in range(4):
                    A, bA = kb.bank((sub % 2) * 4 + cg)
                    for kc in range(16):
                        S.op("pe", lambda e: e.matmul(A, O[:, kc, sub * 128:(sub + 1) * 128], Wo[:, kc, cg * 512:(cg + 1) * 512],
                                                      start=(kc == 0), stop=(kc == 15)), reads=[bO, bWo], writes=[bA])
                    S.op("dve", lambda e: e.scalar_tensor_tensor(out=Z[:, cg * 512:(cg + 1) * 512], in0=X[:, cg * 512:(cg + 1) * 512],
                                                                 scalar=DN_ALPHA, in1=A, op0=ALU.mult, op1=ALU.add),
                         reads=[bX, bA], writes=[bZ])
                layer_norm_tm(kb, Z, bZ, st, mv, rstd, bst, gt, bt, bgb)
                S.dma("sp", xmid[t * 128:(t + 1) * 128, :], Z[:], reads=[bZ], writes=[bxmid])
                if job.get("noroute"):
                    continue
                for q4 in range(4):
                    A, bA = kb.bank(((sub + 1) % 2) * 4 + q4)
                    for j in range(4):
                        kc = q4 * 4 + j
                        S.op("pe", lambda e: e.transpose(A[:, j * 128:(j + 1) * 128], Z[:, kc * 128:(kc + 1) * 128], c["ident"][:]),
                             reads=[bZ, c["buf"]], writes=[bA])
                    S.op("act", lambda e: e.activation(out=xmT[:, q4 * 4:(q4 + 1) * 4, :].rearrange("p a t -> p (a t)"), in_=A,
                                                       func=AF.Copy), reads=[bA], writes=[bxmT])
                A, bA = kb.bank(((sub + 1) % 2) * 4)
                for kc in range(16):
                    S.op("pe", lambda e: e.matmul(A[:, 0:36], xmT[:, kc, :], Wge[:, kc, :], start=(kc == 0), stop=(kc == 15)),
                         reads=[bxmT, bWge], writes=[bA])
                S.op("dve", lambda e: e.tensor_tensor(out=lg[:], in0=A[:, 0:36], in1=bias[:], op=ALU.add),
                     reads=[bA, bbias], writes=[blg])
                route_tile(kb, c, S, r_, br, lg, blg, cnt, bcnt, eC, beC, C, P, t, ((sub + 1) % 2) * 4 + 1)
                for k in range(2):
                    S.idma(xbuf[:, :], bass.IndirectOffsetOnAxis(ap=P["idx"][:, t, k:k + 1], axis=0), Z[:], None,
                           reads=[bZ, P["bidx"]], writes=[bxbuf])
    S.barrier()


def route_tile(kb, c, S, r_, br, lg, blg, cnt, bcnt, eC, beC, C, P, t, bank_i):
    dv = lambda fn, reads, writes: S.op("dve", fn, reads=reads, writes=writes)
    R = [br, blg]
    gl = lg[:, 0:4]
    dv(lambda e: e.tensor_reduce(out=r_["gmax"][:], in_=gl, axis=AX.X, op=ALU.max), R, [br])
    dv(lambda e: e.tensor_scalar(out=r_["gsel"][:], in0=gl, scalar1=r_["gmax"][:, 0:1], scalar2=None, op0=ALU.is_ge), R, [br])
    dv(lambda e: e.tensor_scalar(out=r_["gex"][:], in0=gl, scalar1=r_["gmax"][:, 0:1], scalar2=None, op0=ALU.subtract), R, [br])
    S.op("act", lambda e: e.activation(out=r_["gex"][:], in_=r_["gex"][:], func=AF.Exp), reads=[br], writes=[br])
    dv(lambda e: e.tensor_reduce(out=r_["gsum"][:], in_=r_["gex"][:], axis=AX.X, op=ALU.add), R, [br])
    dv(lambda e: e.reciprocal(out=r_["pg"][:], in_=r_["gsum"][:]), R, [br])
    el = lg[:, 4:36].rearrange("p (g j) -> p g j", j=8)
    for g in range(4):
        dv(lambda e: e.tensor_scalar(out=r_["em"][:, g, :], in0=el[:, g, :], scalar1=r_["gsel"][:, g:g + 1], scalar2=None,
                                     op0=ALU.mult), R, [br])
    dv(lambda e: e.tensor_tensor(out=r_["ein"][:], in0=r_["em"][:, 0, :], in1=r_["em"][:, 1, :], op=ALU.add), R, [br])
    dv(lambda e: e.tensor_tensor(out=r_["ein"][:], in0=r_["ein"][:], in1=r_["em"][:, 2, :], op=ALU.add), R, [br])
    dv(lambda e: e.tensor_tensor(out=r_["ein"][:], in0=r_["ein"][:], in1=r_["em"][:, 3, :], op=ALU.add), R, [br])
    dv(lambda e: e.tensor_reduce(out=r_["m1"][:], in_=r_["ein"][:], axis=AX.X, op=ALU.max), R, [br])
    dv(lambda e: e.tensor_scalar(out=r_["oh1"][:], in0=r_["ein"][:], scalar1=r_["m1"][:, 0:1], scalar2=None, op0=ALU.is_ge), R, [br])
    dv(lambda e: e.scalar_tensor_tensor(out=r_["ein2"][:], in0=r_["oh1"][:], scalar=-1e30, in1=r_["ein"][:],
                                        op0=ALU.mult, op1=ALU.add), R, [br])
    dv(lambda e: e.tensor_reduce(out=r_["m2"][:], in_=r_["ein2"][:], axis=AX.X, op=ALU.max), R, [br])
    dv(lambda e: e.tensor_scalar(out=r_["oh2"][:], in0=r_["ein2"][:], scalar1=r_["m2"][:, 0:1], scalar2=None, op0=ALU.is_ge), R, [br])
    dv(lambda e: e.tensor_tensor(out=r_["dd"][:], in0=r_["m2"][:], in1=r_["m1"][:], op=ALU.subtract), R, [br])
    S.op("act", lambda e: e.activation(out=r_["dd"][:], in_=r_["dd"][:], func=AF.Exp), reads=[br], writes=[br])
    dv(lambda e: e.tensor_scalar(out=r_["dd"][:], in0=r_["dd"][:], scalar1=1.0, scalar2=None, op0=ALU.add), R, [br])
    dv(lambda e: e.reciprocal(out=r_["w1"][:], in_=r_["dd"][:]), R, [br])
    dv(lambda e: e.tensor_scalar(out=r_["w2"][:], in0=r_["w1"][:], scalar1=-1.0, scalar2=1.0, op0=ALU.mult, op1=ALU.add), R, [br])
    dv(lambda e: e.tensor_tensor(out=P["gate"][:, t, 0:1], in0=r_["w1"][:], in1=r_["pg"][:], op=ALU.mult), R, [br, P["bgate"]])
    dv(lambda e: e.tensor_tensor(out=P["gate"][:, t, 1:2], in0=r_["w2"][:], in1=r_["pg"][:], op=ALU.mult), R, [br, P["bgate"]])
    for g in range(4):
        dv(lambda e: e.tensor_scalar(out=r_["M1"][:, g, :], in0=r_["oh1"][:], scalar1=r_["gsel"][:, g:g + 1], scalar2=None,
                                     op0=ALU.mult), R, [br])
        dv(lambda e: e.tensor_scalar(out=r_["M2"][:, g, :], in0=r_["oh2"][:], scalar1=r_["gsel"][:, g:g + 1], scalar2=None,
                                     op0=ALU.mult), R, [br])
    M1f = r_["M1"][:].rearrange("p g j -> p (g j)")
    M2f = r_["M2"][:].rearrange("p g j -> p (g j)")
    dv(lambda e: e.tensor_tensor(out=r_["M"][:], in0=M1f, in1=M2f, op=ALU.add), R, [br])
    A, bA = kb.bank(bank_i)
    S.op("pe", lambda e: e.matmul(A[:, 0:32], c["U"][:], r_["M"][:], start=True, stop=False), reads=[br, c["buf"]], writes=[bA])
    S.op("pe", lambda e: e.matmul(A[:, 0:32], c["ones_f"][0:1, :], cnt[0:1, :], start=False, stop=True),
         reads=[bcnt, c["buf"]], writes=[bA])
    dv(lambda e: e.tensor_scalar(out=r_["rk"][:], in0=A[:, 0:32], scalar1=float(C), scalar2=None, op0=ALU.min), R + [bA], [br])
    dv(lambda e: e.tensor_tensor(out=r_["rk"][:], in0=r_["rk"][:], in1=eC[:], op=ALU.add), R + [beC], [br])
    B2, bB2 = kb.bank(bank_i + 1)
    S.op("pe", lambda e: e.matmul(B2[0:1, 0:32], c["ones_f"][:, 0:1], r_["M"][:], start=True, stop=True),
         reads=[br, c["buf"]], writes=[bB2])
    dv(lambda e: e.tensor_tensor(out=cnt[:], in0=cnt[:], in1=B2[0:1, 0:32], op=ALU.add), [bB2, bcnt], [bcnt])
    for k, Mk in enumerate((M1f, M2f)):
        dv(lambda e: e.tensor_tensor(out=r_["pr"][:], in0=r_["rk"][:], in1=Mk, op=ALU.mult), R, [br])
        dv(lambda e: e.tensor_reduce(out=r_["sl"][:, k:k + 1], in_=r_["pr"][:], axis=AX.X, op=ALU.add), R, [br])
    dv(lambda e: e.tensor_copy(out=P["idx"][:, t, :], in_=r_["sl"][:]), R, [br, P["bidx"]])


def s6_experts(kb, c, job, layer):
    nc, S = kb.nc, kb.S
    n = job["name"]
    C = job["cap"]
    Cs = C + 128
    TS = min(512, C)
    xbuf = kb.dr[f"xbuf_{n}"]
    bxbuf = kb.db[f"xbuf_{n}"]
    ybuf = kb.dr[f"ybuf_{n}"]
    bybuf = kb.db[f"ybuf_{n}"]
    w1, w3, w2 = kb.dr["moe_w1"], kb.dr["moe_w3"], kb.dr["moe_w2"]
    S.barrier()
    with ExitStack() as es:
        def sb(name, shape, dt):
            return es.enter_context(nc.sbuf_tensor(U(name), shape, dt))
        W1 = [sb(f"s6w1{i}", [128, 16, 512], BF16) for i in range(2)]
        W3 = [sb(f"s6w3{i}", [128, 16, 512], BF16) for i in range(2)]
        W2 = [sb(f"s6w2{i}", [128, 4, 2048], BF16) for i in range(2)]
        bW = [Buf(), Buf()]
        xr = [sb(f"s6xr{i}", [128, 2048], F32) for i in range(2)]
        bxr = [Buf(), Buf()]
        xeT = sb("s6xeT", [128, 16, TS], BF16); bxeT = Buf()
        sl = sb("s6sl", [128, TS], F32); bsl = Buf()
        hm = sb("s6hm", [128, 4, TS], BF16); bhm = Buf()
        yst = [sb(f"s6y{i}", [128, 2048], F32) for i in range(2)]
        byst = [Buf(), Buf()]

        def loadw(e, i):
            load_w_bf16(kb, W1[i], bW[i], w1[layer, e], ksplit=4)
            load_w_bf16(kb, W3[i], bW[i], w3[layer, e], ksplit=4)
            load_w_bf16(kb, W2[i], bW[i], w2[layer, e], ksplit=4)
        loadw(0, 0)
        ix = 0
        iy = 0
        ib = 0
        for e in range(job.get("nexp", 32)):
            if e + 1 < 32:
                loadw(e + 1, (e + 1) % 2)
            i = e % 2
            for s0 in range(0, C, TS):
                nsub = TS // 128
                for sub in range(nsub):
                    XR = xr[ix % 2]
                    bXR = bxr[ix % 2]
                    ix += 1
                    r0 = e * Cs + s0 + sub * 128
                    S.dma("pool", XR[:], xbuf[r0:r0 + 128, :], reads=[bxbuf], writes=[bXR])
                    for q4 in range(4):
                        A, bA = kb.bank(ib % 8)
                        ib += 1
                        for j in range(4):
                            kc = q4 * 4 + j
                            S.op("pe", lambda e_: e_.transpose(A[:, j * 128:(j + 1) * 128], XR[:, kc * 128:(kc + 1) * 128], c["ident"][:]),
                                 reads=[bXR, c["buf"]], writes=[bA])
                        dstv = xeT[:, q4 * 4:(q4 + 1) * 4, sub * 128:(sub + 1) * 128]
                        srcv = A.rearrange("p (a t) -> p a t", t=128)
                        if q4 % 2 == 0:
                            S.op("act", lambda e_: e_.activation(out=dstv, in_=srcv, func=AF.Copy), reads=[bA], writes=[bxeT])
                        else:
                            S.op("dve", lambda e_: e_.tensor_copy(out=dstv, in_=srcv), reads=[bA], writes=[bxeT])
                for c4 in range(4):
                    A, bA = kb.bank(ib % 8)
                    ib += 1
                    B, bB = kb.bank(ib % 8)
                    ib += 1
                    for kc in range(16):
                        S.op("pe", lambda e_: e_.matmul(A[:, 0:TS], W1[i][:, kc, c4 * 128:(c4 + 1) * 128], xeT[:, kc, :],
                                                        start=(kc == 0), stop=(kc == 15)), reads=[bW[i], bxeT], writes=[bA])
                    for kc in range(16):
                        S.op("pe", lambda e_: e_.matmul(B[:, 0:TS], W3[i][:, kc, c4 * 128:(c4 + 1) * 128], xeT[:, kc, :],
                                                        start=(kc == 0), stop=(kc == 15)), reads=[bW[i], bxeT], writes=[bB])
                    S.op("act", lambda e_: e_.activation(out=sl[:], in_=A[:, 0:TS], func=AF.Silu), reads=[bA], writes=[bsl])
                    S.op("dve", lambda e_: e_.tensor_tensor(out=hm[:, c4, :], in0=sl[:], in1=B[:, 0:TS], op=ALU.mult),
                         reads=[bsl, bB], writes=[bhm])
                for sub in range(nsub):
                    Y = yst[iy % 2]
                    bY = byst[iy % 2]
                    iy += 1
                    for cg in range(4):
                        A, bA = kb.bank(ib % 8)
                        ib += 1
                        for k4 in range(4):
                            S.op("pe", lambda e_: e_.matmul(A, hm[:, k4, sub * 128:(sub + 1) * 128], W2[i][:, k4, cg * 512:(cg + 1) * 512],
                                                            start=(k4 == 0), stop=(k4 == 3)), reads=[bhm, bW[i]], writes=[bA])
                        if cg % 2 == 0:
                            S.op("act", lambda e_: e_.activation(out=Y[:, cg * 512:(cg + 1) * 512], in_=A, func=AF.Copy),
                                 reads=[bA], writes=[bY])
                        else:
                            S.op("dve", lambda e_: e_.tensor_copy(out=Y[:, cg * 512:(cg + 1) * 512], in_=A), reads=[bA], writes=[bY])
                    r0 = e * Cs + s0 + sub * 128
                    S.dma("pool", ybuf[r0:r0 + 128, :], Y[:], reads=[bY], writes=[bybuf])
    S.barrier()


def s7_combine(kb, c, job, layer, P):
    nc, S = kb.nc, kb.S
    n = job["name"]
    Sn = job["S"]
    NTt = Sn // 128
    xmid = kb.dr[f"xmid_{n}"]
    bxmid = kb.db[f"xmid_{n}"]
    ybuf = kb.dr[f"ybuf_{n}"]
    bybuf = kb.db[f"ybuf_{n}"]
    last = (layer == DEPTH - 1)
    if last:
        dst, bdst = kb.dr[f"y_{n}"], kb.db[f"y_{n}"]
    else:
        dst, bdst = kb.dr[f"x1_{n}"], kb.db[f"x1_{n}"]
        x1T, bx1T = kb.dr[f"x1T_{n}"], kb.db[f"x1T_{n}"]
    S.barrier()
    with ExitStack() as es:
        def sb(name, shape, dt):
            return es.enter_context(nc.sbuf_tensor(U(name), shape, dt))
        gt = sb("s7g", [128, 2048], F32)
        bt = sb("s7b", [128, 2048], F32)
        bgb = Buf()
        load_bcast(kb, gt[:], bgb, kb.dr["ln2_g"][layer])
        load_bcast(kb, bt[:], bgb, kb.dr["ln2_b"][layer])
        xm = [sb(f"s7xm{i}", [128, 2048], F32) for i in range(2)]
        bxm = [Buf(), Buf()]
        r1 = [sb(f"s7r1{i}", [128, 2048], F32) for i in range(2)]
        r2 = [sb(f"s7r2{i}", [128, 2048], F32) for i in range(2)]
        brr = [Buf(), Buf()]
        st = sb("s7st", [128, 4, 6], F32)
        mv = sb("s7mv", [128, 2], F32)
        rstd = sb("s7rstd", [128, 1], F32)
        bst = Buf()
        xTs = [sb(f"s7xT{i}", [128, 16, 128], BF16) for i in range(2)]
        bxTs = [Buf(), Buf()]
        ib = 0
        for t in range(NTt):
            i = t % 2
            Z = xm[i]
            bZ = bxm[i]
            S.dma("sp", Z[:], xmid[t * 128:(t + 1) * 128, :], reads=[bxmid], writes=[bZ])
            S.idma(r1[i][:], None, ybuf[:, :], bass.IndirectOffsetOnAxis(ap=P["idx"][:, t, 0:1], axis=0),
                   reads=[bybuf, P["bidx"]], writes=[brr[i]])
            S.idma(r2[i][:], None, ybuf[:, :], bass.IndirectOffsetOnAxis(ap=P["idx"][:, t, 1:2], axis=0),
                   reads=[bybuf, P["bidx"]], writes=[brr[i]])
            S.op("dve", lambda e: e.tensor_scalar(out=Z[:], in0=Z[:], scalar1=DN_ALPHA, scalar2=None, op0=ALU.mult),
                 reads=[bZ], writes=[bZ])
            S.op("dve", lambda e: e.scalar_tensor_tensor(out=Z[:], in0=r1[i][:], scalar=P["gate"][:, t, 0:1], in1=Z[:],
                                                         op0=ALU.mult, op1=ALU.add), reads=[bZ, brr[i], P["bgate"]], writes=[bZ])
            S.op("dve", lambda e: e.scalar_tensor_tensor(out=Z[:], in0=r2[i][:], scalar=P["gate"][:, t, 1:2], in1=Z[:],
                                                         op0=ALU.mult, op1=ALU.add), reads=[bZ, brr[i], P["bgate"]], writes=[bZ])
            layer_norm_tm(kb, Z, bZ, st, mv, rstd, bst, gt, bt, bgb)
            S.dma("sp", dst[t * 128:(t + 1) * 128, :], Z[:], reads=[bZ], writes=[bdst])
            if not last:
                XT = xTs[i]
                bXT = bxTs[i]
                for q4 in range(4):
                    A, bA = kb.bank(ib % 8)
                    ib += 1
                    for j in range(4):
                        kc = q4 * 4 + j
                        S.op("pe", lambda e: e.transpose(A[:, j * 128:(j + 1) * 128], Z[:, kc * 128:(kc + 1) * 128], c["ident"][:]),
                             reads=[bZ, c["buf"]], writes=[bA])
                    S.op("act", lambda e: e.activation(out=XT[:, q4 * 4:(q4 + 1) * 4, :].rearrange("p a t -> p (a t)"), in_=A,
                                                       func=AF.Copy), reads=[bA], writes=[bXT])
                xv = x1T[:, t * 128:(t + 1) * 128].rearrange("(kc p) t -> p kc t", p=128)
                for k0 in (0, 8):
                    S.dma("sp", xv[:, k0:k0 + 8, :], XT[:, k0:k0 + 8, :], reads=[bXT], writes=[bx1T])
    S.barrier()


def s1_l1(kb, c, job):
    nc, S = kb.nc, kb.S
    n = job["name"]
    Sn = job["S"]
    NT = Sn // 512
    x1T = kb.dr[f"x1T_{n}"]
    bx1T = kb.db[f"x1T_{n}"]
    d = lambda k: (kb.dr[f"{k}_{n}"], kb.db[f"{k}_{n}"])
    qnT, bqnT = d("qnT"); knT, bknT = d("knT"); gqT, bgqT = d("gqT"); qpeT, bqpeT = d("qpeT")
    kpeT, bkpeT = d("kpeT"); gkT, bgkT = d("gkT"); vm, bvm = d("vm"); gv, bgv = d("gv")
    S.barrier()
    with ExitStack() as es:
        def sb(name, shape, dt):
            return es.enter_context(nc.sbuf_tensor(U(name), shape, dt))
        Win = sb("l1win", [128, 16, 2368], BF16); bWin = Buf()
        wi = kb.dr["od_w_in"].rearrange("(kc p) n -> p kc n", p=128)
        for k0 in range(16):
            for c0 in (0, 1184):
                S.dma("pool", Win[:, k0:k0 + 1, c0:c0 + 1184], wi[:, k0:k0 + 1, c0:c0 + 1184], writes=[bWin])
        Wq = sb("l1wq", [128, 4, 1536], BF16); bWq = Buf()
        load_w_bf16(kb, Wq, bWq, kb.dr["mla_w_q_up"], ksplit=4)
        Wkv = sb("l1wkv", [128, 2, 2048], BF16); bWkv = Buf()
        load_w_bf16(kb, Wkv, bWkv, kb.dr["mla_w_kv_up"], ksplit=2)
        ncol = sb("l1ncol", [128, 8], F32); bncol = Buf()

        def colload(j, src1d, off):
            ap = bass.AP(src1d.tensor, src1d.offset + off, [[1, 128], [1, 1]])
            S.dma("sp", ncol[:, j:j + 1], ap, writes=[bncol])
        for j in range(4):
            colload(j, kb.dr["mla_q_norm"][0], j * 128)
        for j in range(2):
            colload(4 + j, kb.dr["mla_kv_norm"][0], j * 128)
        colload(6, kb.dr["gqa_q_norm"][0], 0)
        colload(7, kb.dr["gqa_k_norm"][0], 0)
        X = [sb(f"l1x{i}", [128, 16, 512], BF16) for i in range(2)]
        bX = [Buf(), Buf()]
        tabs = {}
        for nm, pp in (("M_c", 64), ("M_s", 64), ("G_c", 128), ("G_s", 128)):
            tabs[nm] = [sb(f"l1t{nm}{i}", [pp, 512], F32) for i in range(2)]
        btab = [Buf(), Buf()]
        a32 = sb("l1a32", [128, 4, 512], F32); ba32 = Buf()
        sqb = sb("l1sqb", [128, 512], BF16); bsqb = Buf()
        rstd = sb("l1rstd", [128, 512], F32); brstd = Buf()
        qn = sb("l1qn", [128, 4, 512], BF16); bqn = Buf()
        kvn = sb("l1kvn", [128, 2, 512], BF16); bkvn = Buf()
        g32 = sb("l1g32", [128, 512], F32); bg32 = Buf()
        qa = sb("l1qa", [128, 512], BF16); bqa = Buf()
        t1 = sb("l1t1", [128, 512], F32); bt1 = Buf()
        t2 = sb("l1t2", [128, 512], F32); bt2 = Buf()
        og = [sb(f"l1og{i}", [128, 512], BF16) for i in range(3)]
        bog = [Buf() for _ in range(3)]
        vs = [sb(f"l1vs{i}", [128, 1024], BF16) for i in range(2)]
        bvs = [Buf(), Buf()]
        st = {"ib": 0, "io": 0, "iv": 0}
        MS, bMS = kb.bank(7)
        RB, bRB = kb.bank(6)

        def nbank():
            r = kb.bank(st["ib"] % 6)
            st["ib"] += 1
            return r

        def nog():
            i = st["io"] % 3
            st["io"] += 1
            return og[i], bog[i]

        def rms_chunks(cols0, nchunk, ncol0, dim, dst, bdst, Xt, bXt):
            for cc in range(nchunk):
                A, bA = nbank()
                for kc in range(16):
                    S.op("pe", lambda e: e.matmul(A, Win[:, kc, cols0 + cc * 128:cols0 + (cc + 1) * 128], Xt[:, kc, :],
                                                  start=(kc == 0), stop=(kc == 15)), reads=[bWin, bXt], writes=[bA])
                S.op("act", lambda e: e.activation(out=a32[:, cc, :], in_=A, func=AF.Copy), reads=[bA], writes=[ba32])
                S.op("act", lambda e: e.activation(out=sqb[:], in_=a32[:, cc, :], func=AF.Square), reads=[ba32], writes=[bsqb])
                S.op("pe", lambda e: e.matmul(MS, c["ones_b"][:], sqb[:], start=(cc == 0), stop=(cc == nchunk - 1)),
                     reads=[bsqb, c["buf"]], writes=[bMS])
            S.op("dve", lambda e: e.tensor_scalar(out=rstd[:], in0=MS, scalar1=1.0 / dim, scalar2=RMS_EPS,
                                                  op0=ALU.mult, op1=ALU.add), reads=[bMS], writes=[brstd])
            S.op("act", lambda e: e.activation(out=rstd[:], in_=rstd[:], func=AF.Sqrt), reads=[brstd], writes=[brstd])
            S.op("dve", lambda e: e.reciprocal(out=rstd[:], in_=rstd[:]), reads=[brstd], writes=[brstd])
            for cc in range(nchunk):
                S.op("dve", lambda e: e.scalar_tensor_tensor(out=dst[:, cc, :], in0=a32[:, cc, :],
                                                             scalar=ncol[:, ncol0 + cc:ncol0 + cc + 1], in1=rstd[:],
                                                             op0=ALU.mult, op1=ALU.mult),
                     reads=[ba32, brstd, bncol], writes=[bdst])

        for t in range(NT):
            Xt = X[t % 2]
            bXt = bX[t % 2]
            src = x1T[:, t * 512:(t + 1) * 512].rearrange("(kc p) n -> p kc n", p=128)
            for k0 in range(0, 16, 4):
                S.dma("sp", Xt[:, k0:k0 + 4, :], src[:, k0:k0 + 4, :], reads=[bx1T], writes=[bXt])
            bT = btab[t % 2]
            tb = {k: v[t % 2] for k, v in tabs.items()}
            for nm in ("M_c", "M_s", "G_c", "G_s"):
                S.dma("sp", tb[nm][:], kb.dr[f"rope{nm}"][:, t * 512:(t + 1) * 512], writes=[bT])
            cs = slice(t * 512, (t + 1) * 512)
            rms_chunks(0, 4, 0, 512, qn, bqn, Xt, bXt)
            for h in range(8):
                A, bA = nbank()
                for cc in range(4):
                    S.op("pe", lambda e: e.matmul(A, Wq[:, cc, h * 192:h * 192 + 128], qn[:, cc, :], start=(cc == 0), stop=(cc == 3)),
                         reads=[bWq, bqn], writes=[bA])
                O, bO = nog()
                S.op("act", lambda e: e.activation(out=O[:], in_=A, func=AF.Copy), reads=[bA], writes=[bO])
                S.dma("sp", qnT[h, :, cs], O[:], reads=[bO], writes=[bqnT])
                A, bA = nbank()
                for cc in range(4):
                    S.op("pe", lambda e: e.matmul(A[0:64, :], Wq[:, cc, h * 192 + 128:h * 192 + 192], qn[:, cc, :],
                                                  start=(cc == 0), stop=(cc == 3)), reads=[bWq, bqn], writes=[bA])
                O, bO = nog()
                rope_evac(kb, c, A, bA, RB, bRB, c["PM"], 64, tb["M_c"], tb["M_s"], bT, qa, bqa, t1, bt1, t2, bt2, O, bO)
                S.dma("sp", qpeT[h, :, cs], O[0:64, :], reads=[bO], writes=[bqpeT])
            rms_chunks(512, 2, 4, 256, kvn, bkvn, Xt, bXt)
            for h in range(8):
                A, bA = nbank()
                for cc in range(2):
                    S.op("pe", lambda e: e.matmul(A, Wkv[:, cc, h * 256:h * 256 + 128], kvn[:, cc, :], start=(cc == 0), stop=(cc == 1)),
                         reads=[bWkv, bkvn], writes=[bA])
                O, bO = nog()
                S.op("act", lambda e: e.activation(out=O[:], in_=A, func=AF.Copy), reads=[bA], writes=[bO])
                S.dma("sp", knT[h, :, cs], O[:], reads=[bO], writes=[bknT])
            for sub in range(4):
                V = vs[st["iv"] % 2]
                bV = bvs[st["iv"] % 2]
                st["iv"] += 1
                for h in range(8):
                    if h % 4 == 0:
                        A, bA = nbank()
                    for cc in range(2):
                        S.op("pe", lambda e: e.matmul(A[:, (h % 4) * 128:(h % 4 + 1) * 128], kvn[:, cc, sub * 128:(sub + 1) * 128],
                                                      Wkv[:, cc, h * 256 + 128:h * 256 + 256], start=(cc == 0), stop=(cc == 1)),
                             reads=[bWkv, bkvn], writes=[bA])
                    if h % 4 == 3:
                        half = h // 4
                        S.op("act", lambda e: e.activation(out=V[:, half * 512:(half + 1) * 512], in_=A, func=AF.Copy),
                             reads=[bA], writes=[bV])
                r0 = t * 512 + sub * 128
                S.dma("sp", vm[r0:r0 + 128, :], V[:], reads=[bV], writes=[bvm])
            A, bA = nbank()
            for kc in range(16):
                S.op("pe", lambda e: e.matmul(A[0:64, :], Win[:, kc, 768:832], Xt[:, kc, :], start=(kc == 0), stop=(kc == 15)),
                     reads=[bWin, bXt], writes=[bA])
            O, bO = nog()
            rope_evac(kb, c, A, bA, RB, bRB, c["PM"], 64, tb["M_c"], tb["M_s"], bT, qa, bqa, t1, bt1, t2, bt2, O, bO)
            S.dma("sp", kpeT[:, cs], O[0:64, :], reads=[bO], writes=[bkpeT])
            for hh in range(10):
                col0 = 832 + hh * 128
                A, bA = nbank()
                for kc in range(16):
                    S.op("pe", lambda e: e.matmul(A, Win[:, kc, col0:col0 + 128], Xt[:, kc, :], start=(kc == 0), stop=(kc == 15)),
                         reads=[bWin, bXt], writes=[bA])
                S.op("act", lambda e: e.activation(out=g32[:], in_=A, func=AF.Copy), reads=[bA], writes=[bg32])
                S.op("act", lambda e: e.activation(out=sqb[:], in_=g32[:], func=AF.Square), reads=[bg32], writes=[bsqb])
                S.op("pe", lambda e: e.matmul(MS, c["ones_b"][:], sqb[:], start=True, stop=True), reads=[bsqb, c["buf"]], writes=[bMS])
                S.op("dve", lambda e: e.tensor_scalar(out=rstd[:], in0=MS, scalar1=1.0 / 128, scalar2=RMS_EPS,
                                                      op0=ALU.mult, op1=ALU.add), reads=[bMS], writes=[brstd])
                S.op("act", lambda e: e.activation(out=rstd[:], in_=rstd[:], func=AF.Sqrt), reads=[brstd], writes=[brstd])
                S.op("dve", lambda e: e.reciprocal(out=rstd[:], in_=rstd[:]), reads=[brstd], writes=[brstd])
                nc_i = 6 if hh < 8 else 7
                S.op("dve", lambda e: e.scalar_tensor_tensor(out=g32[:], in0=g32[:], scalar=ncol[:, nc_i:nc_i + 1], in1=rstd[:],
                                                             op0=ALU.mult, op1=ALU.mult), reads=[bg32, brstd, bncol], writes=[bg32])
                O, bO = nog()
                rope_evac(kb, c, g32, bg32, RB, bRB, c["PG"], 128, tb["G_c"], tb["G_s"], bT, qa, bqa, t1, bt1, t2, bt2, O, bO)
                if hh < 8:
                    S.dma("sp", gqT[hh, :, cs], O[:], reads=[bO], writes=[bgqT])
                else:
                    S.dma("sp", gkT[hh - 8, :, cs], O[:], reads=[bO], writes=[bgkT])
            for sub in range(4):
                A, bA = nbank()
                for kc in range(16):
                    S.op("pe", lambda e: e.matmul(A[:, 0:256], Xt[:, kc, sub * 128:(sub + 1) * 128], Win[:, kc, 2112:2368],
                                                  start=(kc == 0), stop=(kc == 15)), reads=[bWin, bXt], writes=[bA])
                O, bO = nog()
                S.op("act", lambda e: e.activation(out=O[:, 0:256], in_=A[:, 0:256], func=AF.Copy), reads=[bA], writes=[bO])
                r0 = t * 512 + sub * 128
                S.dma("sp", gv[r0:r0 + 128, :], O[:, 0:256], reads=[bO], writes=[bgv])
    S.barrier()


def _attn_stage(kb, c, job, tag, heads, kv_of, load_head_kv, make_terms, qloads, scale, orow0):
    nc, S = kb.nc, kb.S
    n = job["name"]
    Sn = job["S"]
    NT = Sn // 512
    oT, boT = kb.dr[f"oT_{n}"], kb.db[f"oT_{n}"]
    S.barrier()
    with ExitStack() as es:
        def sb(name, shape, dt):
            return es.enter_context(nc.sbuf_tensor(U(name), shape, dt))
        R = AttnRes(kb, es, tag)
        kv = load_head_kv(sb)
        qbufs = [[sb(f"{tag}q{j}_{i}", [pp, 512], BF16) for i in range(2)] for j, (pp, _) in enumerate(qloads)]
        bq = [Buf(), Buf()]
        om = sb(f"{tag}om", [128, 512], F32); bom = Buf()
        ob = [sb(f"{tag}ob{i}", [128, 512], BF16) for i in range(2)]
        bob = [Buf(), Buf()]
        iq = 0
        cur = None
        for h in range(heads):
            if kv_of(h) != cur:
                cur = kv_of(h)
                kv["load"](cur)
            for t in range(NT):
                i = iq % 2
                Q = [qb[i] for qb in qbufs]
                for (pp, srcfn), q in zip(qloads, Q):
                    S.dma("sp", q[:], srcfn(h)[:, t * 512:(t + 1) * 512], writes=[bq[i]])
                terms = make_terms(kv, Q, bq[i])
                attn_tile(kb, c, R, Sn, terms, kv["v"], kv["bv"], scale, om, bom)
                OB = ob[i]
                S.op("dve", lambda e: e.tensor_copy(out=OB[:], in_=om[:]), reads=[bom], writes=[bob[i]])
                S.dma("sp", oT[orow0 + h * 128:orow0 + (h + 1) * 128, t * 512:(t + 1) * 512], OB[:], reads=[bob[i]], writes=[boT])
                iq += 1
    S.barrier()


def s2_mla(kb, c, job):
    n = job["name"]
    Sn = job["S"]
    S = kb.S
    knT, qnT, qpeT, kpeT, vm = (kb.dr[f"{x}_{n}"] for x in ("knT", "qnT", "qpeT", "kpeT", "vm"))

    def load_head_kv(sb):
        kT = sb("mlkT", [128, Sn], BF16)
        kpe = sb("mlkpe", [64, Sn], BF16)
        v = sb("mlv", [128, Sn // 128, 128], BF16)
        st = {"kT": kT, "kpe": kpe, "v": v, "bk": Buf(), "bv": Buf(), "bkpe": Buf()}
        S.dma("sp", kpe[:], kpeT[:, :], writes=[st["bkpe"]])

        def load(h):
            load_kv(kb, kT, st["bk"], knT[h], v, st["bv"], vm[:, h * 128:(h + 1) * 128], Sn)
        st["load"] = load
        return st

    def make_terms(kv, Q, bQ):
        return [(lambda kc: kv["kT"][:, kc * 128:(kc + 1) * 128], Q[0][:], [kv["bk"], bQ]),
                (lambda kc: kv["kpe"][:, kc * 128:(kc + 1) * 128], Q[1][:], [kv["bkpe"], bQ])]

    _attn_stage(kb, c, job, "ml", 8, lambda h: h, load_head_kv, make_terms,
                [(128, lambda h: qnT[h]), (64, lambda h: qpeT[h])], 192 ** -0.5, 0)


def s3_gqa(kb, c, job):
    n = job["name"]
    Sn = job["S"]
    gqT, gkT, gv = (kb.dr[f"{x}_{n}"] for x in ("gqT", "gkT", "gv"))

    def load_head_kv(sb):
        kT = sb("gqkT", [128, Sn], BF16)
        v = sb("gqv", [128, Sn // 128, 128], BF16)
        st = {"kT": kT, "v": v, "bk": Buf(), "bv": Buf()}

        def load(j):
            load_kv(kb, kT, st["bk"], gkT[j], v, st["bv"], gv[:, j * 128:(j + 1) * 128], Sn)
        st["load"] = load
        return st

    def make_terms(kv, Q, bQ):
        return [(lambda kc: kv["kT"][:, kc * 128:(kc + 1) * 128], Q[0][:], [kv["bk"], bQ])]

    _attn_stage(kb, c, job, "gq", 8, lambda h: h // 4, load_head_kv, make_terms,
                [(128, lambda h: gqT[h])], 128 ** -0.5, 1024)


JOBS = [dict(name="s", S=4096, cap=512), dict(name="p", S=16384, cap=1536)]


def kernel(**inputs):
    jobs = [dict(j) for j in JOBS]
    nc, kb = build_program(jobs)
    used = set(kb.dr.keys())
    base = {}
    for name, shp in WEIGHT_SHAPES.items():
        if name in used:
            base[name] = np.ascontiguousarray(np.asarray(inputs[name], dtype=np.float32).reshape(shp))
    for name, arr in host_consts(max(j["S"] for j in jobs)).items():
        if name in used:
            base[name] = arr
    for j in jobs:
        base[f"ecs_{j['name']}"] = np.ascontiguousarray(
            np.tile((np.arange(32, dtype=np.float32) * (j["cap"] + 128))[None, :], (128, 1)))
    xp = np.asarray(inputs["x_prompt"], dtype=np.float32)[0]
    base["x_p"] = np.ascontiguousarray(xp)
    base["xT_p"] = np.ascontiguousarray(xp.T)
    xs = np.asarray(inputs["x_sample"], dtype=np.float32)
    in_maps = []
    for core in range(8):
        m = dict(base)
        m["x_s"] = np.ascontiguousarray(xs[core % 4])
        m["xT_s"] = np.ascontiguousarray(xs[core % 4].T)
        in_maps.append({k: v for k, v in m.items() if k in used})
    res = run_bass_kernel_spmd(nc, in_maps, core_ids=list(range(8)))
    y_prompt = np.asarray(res.results[0]["y_p"], dtype=np.float32)[None]
    y_sample = np.stack([np.asarray(res.results[cix]["y_s"], dtype=np.float32) for cix in range(4)], 0)
    return (y_prompt, y_sample)
```

```python
import math
from contextlib import ExitStack

import numpy as np
import concourse.bass as bass
import concourse.mybir as mybir
from concourse.bass_utils import run_bass_kernel_spmd

F32 = mybir.dt.float32
BF16 = mybir.dt.bfloat16
I32 = mybir.dt.int32
AF = mybir.ActivationFunctionType
ALU = mybir.AluOpType
AX = mybir.AxisListType

D = 2048
KC = 16
GRID_W = 64
DEPTH = 2
DN_ALPHA = (2 * DEPTH) ** 0.25
LN_EPS = 1e-5
RMS_EPS = 1e-6
NEG = -30000.0
SEM_LIMIT = 30000


_UID = [0]


def U(name):
    _UID[0] += 1
    return f"{name}_u{_UID[0]}"


class Buf:
    __slots__ = ("w", "r")

    def __init__(self):
        self.w = None
        self.r = {}


class _Eng:
    def __init__(self, s, name, eng):
        self.s = s
        self.name = name
        self.eng = eng
        self.sem = s.nc.alloc_semaphore(f"es_{name}_0")
        self.nsem = 1
        self.count = 0
        self.seen = {}

    def bump(self):
        if self.count >= SEM_LIMIT:
            self.sem = self.s.nc.alloc_semaphore(f"es_{self.name}_{self.nsem}")
            self.nsem += 1
            self.count = 0
        self.count += 1
        return (self.sem, self.count, None)


class _DmaSem:
    def __init__(self, s, i):
        self.s = s
        self.i = i
        self.gen = 0
        self.sem = s.nc.alloc_semaphore(f"ds_{i}_0")
        self.count = 0
        self.final = {}

    def bump(self):
        if self.count + 16 > SEM_LIMIT:
            self.final[id(self.sem)] = self.count
            self.gen += 1
            self.sem = self.s.nc.alloc_semaphore(f"ds_{self.i}_{self.gen}")
            self.count = 0
        self.count += 16
        return (self.sem, self.count, self)


class Sched:
    def __init__(self, nc, n_dma_sems=16):
        self.nc = nc
        self.E = {
            "pe": _Eng(self, "pe", nc.tensor),
            "act": _Eng(self, "act", nc.scalar),
            "dve": _Eng(self, "dve", nc.vector),
            "pool": _Eng(self, "pool", nc.gpsimd),
            "sp": _Eng(self, "sp", nc.sync),
        }
        self.dsems = [_DmaSem(self, i) for i in range(n_dma_sems)]
        self.dnext = 0
        self.swsems = [_DmaSem(self, 100 + i) for i in range(8)]
        self.swnext = 0
        self.n_ins = 0
        self.n_wait = 0

    def _wait(self, E, tok):
        sem, val, ds = tok
        if ds is not None:
            val = max(val, ds.count if ds.sem is sem else ds.final[id(sem)])
        key = id(sem)
        if E.seen.get(key, 0) >= val:
            return
        E.eng.wait_ge(sem, val)
        E.seen[key] = val
        self.n_wait += 1

    def _deps(self, E, reads, writes, skip_same):
        for b in reads:
            t = b.w
            if t is not None and not (skip_same and t[2] is None and t[0] is E.sem):
                self._wait(E, t)
        for b in writes:
            t = b.w
            if t is not None and not (skip_same and t[2] is None and t[0] is E.sem):
                self._wait(E, t)
            for t in b.r.values():
                if not (skip_same and t[2] is None and t[0] is E.sem):
                    self._wait(E, t)

    def _mark(self, tok, reads, writes):
        k = id(tok[0])
        for b in reads:
            b.r[k] = tok
        for b in writes:
            b.w = tok
            b.r = {}

    def op(self, eng, fn, reads=(), writes=()):
        E = self.E[eng]
        self._deps(E, reads, writes, eng == "pe")
        ins = fn(E.eng)
        tok = E.bump()
        ins.then_inc(tok[0], 1)
        self._mark(tok, reads, writes)
        self.n_ins += 1
        return ins

    def dma(self, q, out, in_, reads=(), writes=(), **kw):
        E = self.E[q]
        self._deps(E, reads, writes, False)
        if q == "pool":
            ds = self.swsems[self.swnext]
            self.swnext = (self.swnext + 1) % len(self.swsems)
            if ds.count > 0:
                self._wait(E, (ds.sem, ds.count, ds))
        else:
            ds = self.dsems[self.dnext]
            self.dnext = (self.dnext + 1) % len(self.dsems)
        ins = E.eng.dma_start(out=out, in_=in_, **kw)
        tok = ds.bump()
        ins.then_inc(tok[0], 16)
        self._mark(tok, reads, writes)
        self.n_ins += 1
        return ins

    def idma(self, out, out_off, in_, in_off, reads=(), writes=(), **kw):
        E = self.E["pool"]
        self._deps(E, reads, writes, False)
        ds = self.swsems[self.swnext]
        self.swnext = (self.swnext + 1) % len(self.swsems)
        if ds.count > 0:
            self._wait(E, (ds.sem, ds.count, ds))
        ins = E.eng.indirect_dma_start(out, out_off, in_, in_off, **kw)
        tok = ds.bump()
        ins.then_inc(tok[0], 16)
        self._mark(tok, reads, writes)
        self.n_ins += 1
        return ins

    def barrier(self):
        for E in self.E.values():
            for F in self.E.values():
                if F is not E and F.count > 0:
                    self._wait(E, (F.sem, F.count, None))
            for ds in self.dsems + self.swsems:
                if ds.count > 0:
                    self._wait(E, (ds.sem, ds.count, ds))


def bcast_rows(ap2d_row, nparts):
    t = ap2d_row
    return bass.AP(t.tensor, t.offset, [[0, nparts]] + [list(x) for x in t.ap[1:]])


class KB:
    def __init__(self, nc):
        self.nc = nc
        self.S = Sched(nc)
        self.pq = [nc.alloc_psum_tensor(f"pq{i}", [128, 1024], F32) for i in range(4)]
        self.pb = [Buf() for _ in range(8)]
        self.reg = {}
        self.db = {}
        kb = self

        class _Lazy(dict):
            def __missing__(d, name):
                shp = kb.reg[name]
                t = kb.nc.dram_tensor(name, list(shp), F32, kind="ExternalInput")
                d[name] = t.ap()
                kb.db[name] = Buf()
                return d[name]
        self.dr = _Lazy()

    def bank(self, i):
        return self.pq[i // 2][:, (i % 2) * 512:(i % 2) * 512 + 512], self.pb[i]

    def dram(self, name, shape, dt, kind="Internal"):
        t = self.nc.dram_tensor(name, list(shape), dt, kind=kind)
        self.dr[name] = t.ap()
        self.db[name] = Buf()
        return self.dr[name]


def load_consts(kb, es):
    nc, S = kb.nc, kb.S
    c = {}
    cb = Buf()

    def sb(name, shape, dt):
        return es.enter_context(nc.sbuf_tensor(U(name), shape, dt))

    c["ident"] = sb("k_ident", [128, 128], F32)
    c["ones_f"] = sb("k_onesf", [128, 128], F32)
    c["ones_b"] = sb("k_onesb", [128, 128], BF16)
    c["PA"] = sb("k_PA", [128, 128], BF16)
    c["PG"] = sb("k_PG", [128, 128], BF16)
    c["PM"] = sb("k_PM", [64, 64], BF16)
    c["U"] = sb("k_U", [128, 128], F32)
    c["J"] = sb("k_J", [64, 64], F32)
    c["mneg"] = sb("k_mneg", [128, 64], F32)
    S.dma("sp", c["ident"][:], kb.dr["c_ident"][:, :], writes=[cb])
    S.dma("sp", c["U"][:], kb.dr["c_U"][:, :], writes=[cb])
    S.dma("sp", c["J"][:], kb.dr["c_J"][:, :], writes=[cb])
    S.dma("sp", c["mneg"][:], kb.dr["c_mneg"][:, :], writes=[cb])
    S.dma("pool", c["PA"][:], kb.dr["c_PA"][:, :], writes=[cb])
    S.dma("pool", c["PG"][:], kb.dr["c_PG"][:, :], writes=[cb])
    S.dma("pool", c["PM"][:], kb.dr["c_PM"][:, :], writes=[cb])
    S.op("dve", lambda e: e.memset(c["ones_f"][:], 1.0), writes=[cb])
    S.op("dve", lambda e: e.memset(c["ones_b"][:], 1.0), writes=[cb])
    c["buf"] = cb
    return c


def load_w_bf16(kb, dst, dbuf, src2d, ksplit=4):
    K = src2d.shape[0]
    kc = K // 128
    v = src2d.rearrange("(kc p) n -> p kc n", p=128)
    step = max(1, kc // ksplit)
    for k0 in range(0, kc, step):
        kb.S.dma("pool", dst[:, k0:k0 + step, :], v[:, k0:k0 + step, :], writes=[dbuf])


def rope_evac(kb, c, A, bA, Bk, bB, Pm, np_, ctab, stab, btab, qa, bqa, t1, bt1, t2, bt2, out, bout):
    S = kb.S
    import os
    R_ = int(os.environ.get("ROPE_STEPS", "5"))
    S.op("act", lambda e: e.activation(out=qa[:np_, :], in_=A[:np_, :], func=AF.Copy), reads=[bA], writes=[bqa])
    if R_ < 2:
        return
    S.op("pe", lambda e: e.matmul(Bk[:np_, :], Pm[:np_, :np_], qa[:np_, :], start=True, stop=True),
         reads=[bqa, c["buf"]], writes=[bB])
    if R_ < 3:
        return
    S.op("act", lambda e: e.activation(out=t1[:np_, :], in_=A[:np_, :], func=AF.Copy), reads=[bA], writes=[bt1])
    S.op("dve", lambda e: e.tensor_tensor(out=t1[:np_, :], in0=t1[:np_, :], in1=ctab[:np_, :], op=ALU.mult),
         reads=[btab, bt1], writes=[bt1])
    if R_ < 4:
        return
    S.op("dve", lambda e: e.tensor_tensor(out=t2[:np_, :], in0=Bk[:np_, :], in1=stab[:np_, :], op=ALU.mult),
         reads=[bB, btab], writes=[bt2])
    if R_ < 5:
        return
    S.op("dve", lambda e: e.tensor_tensor(out=out[:np_, :], in0=t1[:np_, :], in1=t2[:np_, :], op=ALU.add),
         reads=[bt1, bt2], writes=[bout])


def s1_l0(kb, c, job):
    nc, S = kb.nc, kb.S
    n = job["name"]
    Sn = job["S"]
    NT = Sn // 512
    xT = kb.dr[f"xT_{n}"]
    w_in = kb.dr["ev_w_in"]
    S.barrier()
    with ExitStack() as es:
        def sb(name, shape, dt):
            return es.enter_context(nc.sbuf_tensor(U(name), shape, dt))
        wg = [sb(f"s1wg{i}", [128, 16, 1024], BF16) for i in range(2)]
        bwg = [Buf(), Buf()]
        xt = [sb(f"s1xt{i}", [128, 16, 512], BF16) for i in range(2)]
        bxt = [Buf(), Buf()]
        ct = [sb(f"s1ct{i}", [128, 512], F32) for i in range(2)]
        st = [sb(f"s1st{i}", [128, 512], F32) for i in range(2)]
        btab = [Buf(), Buf()]
        qa = sb("s1qa", [128, 512], BF16); bqa = Buf()
        t1 = sb("s1t1", [128, 512], F32); bt1 = Buf()
        t2 = sb("s1t2", [128, 512], F32); bt2 = Buf()
        og = [sb(f"s1og{i}", [128, 512], BF16) for i in range(3)]
        bog = [Buf() for _ in range(3)]
        vs = [sb(f"s1vs{i}", [128, 1024], BF16) for i in range(2)]
        bvs = [Buf(), Buf()]
        gnames = ["qTa", "kTa", "va", "qTb", "kTb", "vb"]
        it = 0
        io = 0
        iv = 0
        ib = 0
        groups = job.get("groups", list(range(6)))
        load_w_bf16(kb, wg[0], bwg[0], w_in[:, groups[0] * 1024:(groups[0] + 1) * 1024])
        for gi, g in enumerate(groups):
            if gi + 1 < len(groups):
                g1 = groups[gi + 1]
                load_w_bf16(kb, wg[(gi + 1) % 2], bwg[(gi + 1) % 2], w_in[:, g1 * 1024:(g1 + 1) * 1024])
            W = wg[gi % 2]
            bW = bwg[gi % 2]
            dst = kb.dr[f"{gnames[g]}_{n}"]
            bdst = kb.db[f"{gnames[g]}_{n}"]
            for t in range(NT):
                X = xt[it % 2]
                bX = bxt[it % 2]
                src = xT[:, t * 512:(t + 1) * 512].rearrange("(kc p) n -> p kc n", p=128)
                for k0 in range(0, 16, 8):
                    S.dma("pool", X[:, k0:k0 + 8, :], src[:, k0:k0 + 8, :], writes=[bX])
                if g in (0, 1):
                    C_ = ct[it % 2]
                    S_ = st[it % 2]
                    bT = btab[it % 2]
                    S.dma("sp", C_[:], kb.dr["ropeA_c"][:, t * 512:(t + 1) * 512], writes=[bT])
                    S.dma("sp", S_[:], kb.dr["ropeA_s"][:, t * 512:(t + 1) * 512], writes=[bT])
                it += 1
                if g in (0, 1, 3, 4):
                    for h in range(job.get("heads", 8)):
                        A, bA = kb.bank(ib % 6)
                        ib += 1
                        for kc in range(16):
                            S.op("pe", lambda e: e.matmul(A, W[:, kc, h * 128:(h + 1) * 128], X[:, kc, :],
                                                          start=(kc == 0), stop=(kc == 15)),
                                 reads=[bW, bX], writes=[bA])
                        O = og[io % 3]
                        bO = bog[io % 3]
                        io += 1
                        if g in (0, 1):
                            Bk, bB = kb.bank(6 + (ib % 2))
                            rope_evac(kb, c, A, bA, Bk, bB, c["PA"], 128, C_, S_, bT, qa, bqa, t1, bt1, t2, bt2, O, bO)
                        else:
                            S.op("act", lambda e: e.activation(out=O[:], in_=A, func=AF.Copy), reads=[bA], writes=[bO])
                        if not job.get("nostore"):
                            S.dma("sp", dst[h, :, t * 512:(t + 1) * 512], O[:], reads=[bO], writes=[bdst])
                else:
                    for sub in range(4):
                        V = vs[iv % 2]
                        bV = bvs[iv % 2]
                        iv += 1
                        for half in range(2):
                            A, bA = kb.bank(ib % 6)
                            ib += 1
                            for kc in range(16):
                                S.op("pe", lambda e: e.matmul(A, X[:, kc, sub * 128:(sub + 1) * 128],
                                                              W[:, kc, half * 512:(half + 1) * 512],
                                                              start=(kc == 0), stop=(kc == 15)),
                                     reads=[bW, bX], writes=[bA])
                            if half == 0:
                                S.op("act", lambda e: e.activation(out=V[:, 0:512], in_=A, func=AF.Copy),
                                     reads=[bA], writes=[bV])
                            else:
                                S.op("dve", lambda e: e.tensor_copy(out=V[:, 512:1024], in_=A),
                                     reads=[bA], writes=[bV])
                        r0 = t * 512 + sub * 128
                        S.dma("sp", dst[r0:r0 + 128, :], V[:], reads=[bV], writes=[bdst])
    S.barrier()


class AttnRes:
    def __init__(self, kb, es, tag):
        nc = kb.nc
        self.pT = [es.enter_context(nc.sbuf_tensor(U(f"{tag}pT{i}"), [128, 1024], BF16)) for i in range(3)]
        self.bpT = [Buf() for _ in range(3)]
        self.accs = es.enter_context(nc.sbuf_tensor(U(f"{tag}accs"), [128, 1024], F32))
        self.baccs = Buf()
        self.rs = es.enter_context(nc.sbuf_tensor(U(f"{tag}rs"), [128, 512], F32))
        self.brs = Buf()
        self.i = 0


def attn_tile(kb, c, R, Sn, terms, v, bv, scale, om, bom):
    S = kb.S
    OT, bOT = kb.bank(6)
    MS, bMS = kb.bank(7)
    nb = Sn // 256
    nkc = Sn // 128

    def qk(b2):
        i = R.i % 3
        R.i += 1
        ps = kb.pq[i]
        bps = [kb.pb[2 * i], kb.pb[2 * i + 1]]
        for j in range(2):
            kc = 2 * b2 + j
            for ti, (kfn, q, tb) in enumerate(terms):
                S.op("pe", lambda e: e.matmul(ps[:, j * 512:(j + 1) * 512], kfn(kc), q,
                                              start=(ti == 0), stop=(ti == len(terms) - 1)),
                     reads=tb, writes=[bps[j]])
        return i

    def rest(b2, i):
        ps = kb.pq[i]
        bps = [kb.pb[2 * i], kb.pb[2 * i + 1]]
        pT = R.pT[i]
        bpT = R.bpT[i]
        S.op("act", lambda e: e.activation(out=pT[:], in_=ps[:, :], func=AF.Exp, scale=scale),
             reads=bps, writes=[bpT])
        for j in range(2):
            kc = 2 * b2 + j
            S.op("pe", lambda e: e.matmul(OT, v[:, kc, :], pT[:, j * 512:(j + 1) * 512],
                                          start=(kc == 0), stop=(kc == nkc - 1)),
                 reads=[bv, bpT], writes=[bOT])
        if b2 == 0:
            S.op("dve", lambda e: e.tensor_copy(out=R.accs[:], in_=pT[:]), reads=[bpT], writes=[R.baccs])
        else:
            S.op("dve", lambda e: e.tensor_tensor(out=R.accs[:], in0=R.accs[:], in1=pT[:], op=ALU.add),
                 reads=[bpT, R.baccs], writes=[R.baccs])

    cur = qk(0)
    for b2 in range(nb):
        nxt = qk(b2 + 1) if b2 + 1 < nb else None
        rest(b2, cur)
        cur = nxt
    for j in range(2):
        S.op("pe", lambda e: e.matmul(MS, c["ones_f"][:], R.accs[:, j * 512:(j + 1) * 512],
                                      start=(j == 0), stop=(j == 1)),
             reads=[R.baccs, c["buf"]], writes=[bMS])
    S.op("dve", lambda e: e.reciprocal(out=R.rs[:], in_=MS), reads=[bMS], writes=[R.brs])
    S.op("dve", lambda e: e.tensor_tensor(out=om[:], in0=OT, in1=R.rs[:], op=ALU.mult),
         reads=[bOT, R.brs], writes=[bom])


def load_kv(kb, kT, bkT, ksrc, v, bv, vsrc_cols, Sn):
    S = kb.S
    nsp = 4 if Sn >= 2048 else 1
    w = Sn // nsp
    for i in range(nsp):
        S.dma("sp", kT[:, i * w:(i + 1) * w], ksrc[:, i * w:(i + 1) * w], writes=[bkT])
    vv = vsrc_cols.rearrange("(c p) d -> p c d", p=128)
    nc_ = Sn // 128
    wv = min(8, nc_)
    for i in range(0, nc_, wv):
        S.dma("sp", v[:, i:i + wv, :], vv[:, i:i + wv, :], writes=[bv])


def s2_diff(kb, c, job):
    nc, S = kb.nc, kb.S
    n = job["name"]
    Sn = job["S"]
    NT = Sn // 512
    qTa, kTa, va, oT = (kb.dr[f"{x}_{n}"] for x in ("qTa", "kTa", "va", "oT"))
    boT = kb.db[f"oT_{n}"]
    lam_init = 0.8 - 0.6 * math.exp(-0.3 * 0)
    S.barrier()
    with ExitStack() as es:
        def sb(name, shape, dt):
            return es.enter_context(nc.sbuf_tensor(U(name), shape, dt))
        R = AttnRes(kb, es, "s2")
        kT = sb("s2kT", [128, Sn], BF16); bkT = Buf()
        v = sb("s2v", [128, Sn // 128, 128], BF16); bv = Buf()
        qT = [sb(f"s2q{i}", [128, 512], BF16) for i in range(2)]
        bq = [Buf(), Buf()]
        om = [sb(f"s2om{i}", [128, 512], F32) for i in range(2)]
        bom = [Buf(), Buf()]
        o = sb("s2o", [128, 512], F32); bo = Buf()
        sq = sb("s2sq", [128, 512], F32); bsq = Buf()
        rstd = sb("s2rstd", [128, 512], F32); brstd = Buf()
        ob = [sb(f"s2ob{i}", [128, 512], BF16) for i in range(2)]
        bob = [Buf(), Buf()]
        lv = sb("s2lv", [128, 4, 64], F32); blv = Buf()
        pr = sb("s2pr", [128, 2, 64], F32); bpr = Buf()
        sm = sb("s2sm", [128, 2], F32); bsm = Buf()
        ex = sb("s2ex", [128, 2], F32); bex = Buf()
        nlam = sb("s2nlam", [128, 1], F32); bnl = Buf()
        gcol = sb("s2gcol", [128, 1], F32); bg = Buf()
        dl = kb.dr["diff_lambda"]
        S.dma("sp", lv[:].rearrange("p a b -> p (a b)"),
              bcast_rows(dl.rearrange("a b -> (a b)")[None, :], 128) if False else
              bass.AP(dl.tensor, dl.offset, [[0, 128], [1, 256]]), writes=[blv])
        S.op("dve", lambda e: e.tensor_tensor(out=pr[:, 0, :], in0=lv[:, 0, :], in1=lv[:, 1, :], op=ALU.mult),
             reads=[blv], writes=[bpr])
        S.op("dve", lambda e: e.tensor_tensor(out=pr[:, 1, :], in0=lv[:, 2, :], in1=lv[:, 3, :], op=ALU.mult),
             reads=[blv], writes=[bpr])
        S.op("dve", lambda e: e.tensor_reduce(out=sm[:], in_=pr[:], axis=AX.X, op=ALU.add), reads=[bpr], writes=[bsm])
        S.op("act", lambda e: e.activation(out=ex[:], in_=sm[:], func=AF.Exp), reads=[bsm], writes=[bex])
        S.op("dve", lambda e: e.tensor_tensor(out=nlam[:], in0=ex[:, 1:2], in1=ex[:, 0:1], op=ALU.subtract),
             reads=[bex], writes=[bnl])
        S.op("dve", lambda e: e.tensor_scalar(out=nlam[:], in0=nlam[:], scalar1=-lam_init, scalar2=None, op0=ALU.add),
             reads=[bnl], writes=[bnl])
        g = kb.dr["diff_subln"]
        S.dma("sp", gcol[:], bass.AP(g.tensor, g.offset, [[1, 128], [1, 1]]), writes=[bg])
        S.op("dve", lambda e: e.tensor_scalar(out=gcol[:], in0=gcol[:], scalar1=(1.0 - lam_init), scalar2=None,
                                              op0=ALU.mult), reads=[bg], writes=[bg])
        iq = 0
        MS, bMS = kb.bank(7)
        for h in range(8):
            load_kv(kb, kT, bkT, kTa[h], v, bv, va[:, h * 128:(h + 1) * 128], Sn)
            for t in range(NT):
                Q = qT[iq % 2]
                bQ = bq[iq % 2]
                S.dma("sp", Q[:], qTa[h, :, t * 512:(t + 1) * 512], writes=[bQ])
                for m in range(2):
                    lo = 64 * m
                    terms = [(lambda kc, lo=lo: kT[lo:lo + 64, kc * 128:(kc + 1) * 128], Q[lo:lo + 64, :], [bkT, bQ])]
                    attn_tile(kb, c, R, Sn, terms, v, bv, 0.125, om[m], bom[m])
                S.op("dve", lambda e: e.scalar_tensor_tensor(out=o[:], in0=om[1][:], scalar=nlam[:, 0:1], in1=om[0][:],
                                                             op0=ALU.mult, op1=ALU.add),
                     reads=[bom[0], bom[1], bnl], writes=[bo])
                S.op("act", lambda e: e.activation(out=sq[:], in_=o[:], func=AF.Square), reads=[bo], writes=[bsq])
                S.op("pe", lambda e: e.matmul(MS, c["ones_f"][:], sq[:], start=True, stop=True),
                     reads=[bsq, c["buf"]], writes=[bMS])
                S.op("dve", lambda e: e.tensor_scalar(out=rstd[:], in0=MS, scalar1=1.0 / 128, scalar2=1e-5,
                                                      op0=ALU.mult, op1=ALU.add), reads=[bMS], writes=[brstd])
                S.op("act", lambda e: e.activation(out=rstd[:], in_=rstd[:], func=AF.Sqrt), reads=[brstd], writes=[brstd])
                S.op("dve", lambda e: e.reciprocal(out=rstd[:], in_=rstd[:]), reads=[brstd], writes=[brstd])
                S.op("dve", lambda e: e.tensor_tensor(out=o[:], in0=o[:], in1=rstd[:], op=ALU.mult),
                     reads=[bo, brstd], writes=[bo])
                OB = ob[iq % 2]
                bOB = bob[iq % 2]
                S.op("dve", lambda e: e.tensor_scalar(out=OB[:], in0=o[:], scalar1=gcol[:, 0:1], scalar2=None,
                                                      op0=ALU.mult), reads=[bo, bg], writes=[bOB])
                S.dma("sp", oT[h * 128:(h + 1) * 128, t * 512:(t + 1) * 512], OB[:], reads=[bOB], writes=[boT])
                iq += 1
    S.barrier()


def host_consts(smax):
    pos = np.arange(smax, dtype=np.float32)
    out = {}
    inv = (500000.0 ** (-np.arange(0, 16, 2, dtype=np.float32) / 16)).astype(np.float32)
    ang = pos[None, :] * inv[:, None]
    cA = np.ones((128, smax), np.float32)
    sA = np.zeros((128, smax), np.float32)
    PA = np.zeros((128, 128), np.float32)
    for m in range(2):
        for j in range(8):
            a, b = 64 * m + j, 64 * m + j + 8
            cA[a] = np.cos(ang[j]); cA[b] = np.cos(ang[j])
            sA[a] = -np.sin(ang[j]); sA[b] = np.sin(ang[j])
            PA[a, b] = 1.0; PA[b, a] = 1.0
    out["ropeA_c"], out["ropeA_s"], out["c_PA"] = cA, sA, PA
    inv = (10000.0 ** (-np.arange(0, 64, 2, dtype=np.float32) / 64)).astype(np.float32)
    ang = pos[None, :] * inv[:, None]
    cM = np.concatenate([np.cos(ang), np.cos(ang)], 0).astype(np.float32)
    sM = np.concatenate([-np.sin(ang), np.sin(ang)], 0).astype(np.float32)
    PM = np.zeros((64, 64), np.float32)
    for j in range(32):
        PM[j, j + 32] = 1.0; PM[j + 32, j] = 1.0
    out["ropeM_c"], out["ropeM_s"], out["c_PM"] = cM, sM, PM
    angr = (pos // GRID_W)[None, :] * inv[:, None]
    angc = (pos % GRID_W)[None, :] * inv[:, None]
    cG = np.concatenate([np.cos(angr), np.cos(angr), np.cos(angc), np.cos(angc)], 0).astype(np.float32)
    sG = np.concatenate([-np.sin(angr), np.sin(angr), -np.sin(angc), np.sin(angc)], 0).astype(np.float32)
    PG = np.zeros((128, 128), np.float32)
    PG[:64, :64] = PM
    PG[64:, 64:] = PM
    out["ropeG_c"], out["ropeG_s"], out["c_PG"] = cG, sG, PG
    out["c_ident"] = np.eye(128, dtype=np.float32)
    out["c_U"] = np.triu(np.ones((128, 128), np.float32), 1)
    out["c_J"] = np.eye(64, dtype=np.float32)[::-1].copy()
    cols = np.arange(64)
    cs = np.clip(cols - 8, 0, 48)
    m = np.full((64, 64), NEG, np.float32)
    for cq in range(64):
        m[cs[cq]:cs[cq] + 16, cq] = 0.0
    out["c_mneg"] = np.concatenate([m, m], 0)
    return out


WEIGHT_SHAPES = {
    "ev_w_in": [2048, 6144], "ev_w_out": [2048, 2048], "diff_lambda": [4, 64], "diff_subln": [1, 128],
    "na_rpb": [8, 15, 31], "od_w_in": [2048, 2368], "od_w_out": [2048, 2048], "mla_q_norm": [1, 512],
    "mla_w_q_up": [512, 1536], "mla_kv_norm": [1, 256], "mla_w_kv_up": [256, 2048], "gqa_q_norm": [1, 128],
    "gqa_k_norm": [1, 128], "ln1_g": [2, 2048], "ln1_b": [2, 2048], "ln2_g": [2, 2048], "ln2_b": [2, 2048],
    "moe_w_group": [2, 2048, 4], "moe_b_group": [2, 4], "moe_w_expert": [2, 2048, 32], "moe_b_expert": [2, 32],
    "moe_w1": [2, 32, 2048, 512], "moe_w3": [2, 32, 2048, 512], "moe_w2": [2, 32, 512, 2048],
}


def build_program(jobs, stages=None, debug=False, wnames=None):
    nc = bass.Bass("TRN2", target_bir_lowering=False)
    kb = KB(nc)
    smax = max(j["S"] for j in jobs)
    skind = "ExternalOutput" if debug else "Internal"
    for name, shp in WEIGHT_SHAPES.items():
        kb.reg[name] = shp
    hc = host_consts(128)
    for name, arr in hc.items():
        shp = list(arr.shape)
        if name.startswith("rope"):
            shp[1] = smax
        kb.reg[name] = shp
    kb.dram("rpbpad", [8, 15, 160], F32)
    for j in jobs:
        n, Sn = j["name"], j["S"]
        Cs = j["cap"] + 128
        kb.reg[f"x_{n}"] = [Sn, D]
        kb.reg[f"xT_{n}"] = [D, Sn]
        kb.reg[f"ecs_{n}"] = [128, 32]
        kb.dram(f"y_{n}", [Sn, D], F32, kind="ExternalOutput")
        for t in ("qTa", "kTa", "qTb", "kTb"):
            kb.dram(f"{t}_{n}", [8, 128, Sn], BF16, kind=skind)
        kb.dram(f"va_{n}", [Sn, 1024], BF16, kind=skind)
        kb.dram(f"vb_{n}", [Sn, 1024], BF16, kind=skind)
        kb.dram(f"oT_{n}", [D, Sn], BF16, kind=skind)
        kb.dram(f"xmid_{n}", [Sn, D], F32, kind=skind)
        kb.dram(f"xbuf_{n}", [32 * Cs, D], BF16)
        kb.dram(f"ybuf_{n}", [32 * Cs, D], BF16)
        kb.dram(f"x1_{n}", [Sn, D], F32, kind=skind)
        kb.dram(f"x1T_{n}", [D, Sn], BF16, kind=skind)
        for t in ("qnT", "knT", "gqT"):
            kb.dram(f"{t}_{n}", [8, 128, Sn], BF16, kind=skind)
        kb.dram(f"qpeT_{n}", [8, 64, Sn], BF16, kind=skind)
        kb.dram(f"kpeT_{n}", [64, Sn], BF16, kind=skind)
        kb.dram(f"gkT_{n}", [2, 128, Sn], BF16, kind=skind)
        kb.dram(f"vm_{n}", [Sn, 1024], BF16, kind=skind)
        kb.dram(f"gv_{n}", [Sn, 256], BF16, kind=skind)
    def on(x):
        return stages is None or x in stages
    with ExitStack() as es:
        c = load_consts(kb, es)
        for j in jobs:
            n, Sn = j["name"], j["S"]
            P = {"idx": es.enter_context(nc.sbuf_tensor(f"P_idx_{n}", [128, Sn // 128, 2], I32)),
                 "gate": es.enter_context(nc.sbuf_tensor(f"P_gate_{n}", [128, Sn // 128, 2], F32)),
                 "bidx": Buf(), "bgate": Buf()}
            if on("s1"):
                s1_l0(kb, c, j)
            if on("s2"):
                s2_diff(kb, c, j)
            if on("s3"):
                s3_na(kb, c, j)
            if on("s4"):
                s45_post(kb, c, j, 0, P)
            if on("s6"):
                s6_experts(kb, c, j, 0)
            if on("s7"):
                s7_combine(kb, c, j, 0, P)
            if on("l1"):
                s1_l1(kb, c, j)
                s2_mla(kb, c, j)
                s3_gqa(kb, c, j)
                s45_post(kb, c, j, 1, P)
                s6_experts(kb, c, j, 1)
                s7_combine(kb, c, j, 1, P)
        kb.S.barrier()
    return nc, kb


def s3_na(kb, c, job):
    nc, S = kb.nc, kb.S
    n = job["name"]
    Sn = job["S"]
    rows = Sn // GRID_W
    NT = Sn // 512
    qTb, kTb, vb, oT = (kb.dr[f"{x}_{n}"] for x in ("qTb", "kTb", "vb", "oT"))
    boT = kb.db[f"oT_{n}"]
    rpb = kb.dr["na_rpb"]
    pad = kb.dr["rpbpad"]
    bpad = kb.db["rpbpad"]
    scale = 128 ** -0.5

    def rs(r):
        return min(max(r - 4, 0), rows - 8)

    S.barrier()
    with ExitStack() as es:
        def sb(name, shape, dt):
            return es.enter_context(nc.sbuf_tensor(U(name), shape, dt))
        z = sb("s3z", [120, 160], F32); bz = Buf()
        S.op("dve", lambda e: e.memset(z[:], 0.0), writes=[bz])
        S.dma("sp", z[:, 64:95], rpb.rearrange("h a w -> (h a) w"), writes=[bz])
        S.dma("sp", pad.rearrange("h a w -> (h a) w"), z[:], reads=[bz], writes=[bpad])
        kT = sb("s3kT", [128, Sn], BF16); bkT = Buf()
        v = sb("s3v", [128, Sn // 128, 128], BF16); bv = Buf()
        tpp = sb("s3tpp", [64, 15, 128], F32); btpp = Buf()
        Tm = sb("s3Tm", [128, 15, 64], F32); bTm = Buf()
        NPAT = 8
        Bt = [sb(f"s3Bt{i}", [128, 5, 128], F32) for i in range(NPAT)]
        bBt = [Buf() for _ in range(NPAT)]
        qT = [sb(f"s3q{i}", [128, 512], BF16) for i in range(2)]
        bq = [Buf(), Buf()]
        sc = [sb(f"s3sc{i}", [128, 640], F32) for i in range(2)]
        bsc = [Buf(), Buf()]
        pT = [sb(f"s3pT{i}", [128, 640], BF16) for i in range(2)]
        bpT = [Buf(), Buf()]
        rsum = sb("s3rs", [128, 512], F32); brs = Buf()
        ob = [sb(f"s3ob{i}", [128, 512], BF16) for i in range(2)]
        bob = [Buf(), Buf()]
        OT, bOT = kb.bank(6)
        MS, bMS = kb.bank(7)
        ib = 0
        iq = 0
        for h in range(8):
            load_kv(kb, kT, bkT, kTb[h], v, bv, vb[:, h * 128:(h + 1) * 128], Sn)
            base = pad[h, 0, 16:17]
            src = bass.AP(base.tensor, base.offset, [[1, 64], [160, 15], [1, 64]])
            S.dma("sp", tpp[:, :, 0:64], src, reads=[bpad], writes=[btpp])
            S.dma("sp", tpp[:, :, 64:128], src, reads=[bpad], writes=[btpp])
            for half in range(2):
                pb_ap = kb.pq[half]
                pbb = [kb.pb[2 * half], kb.pb[2 * half + 1]]
                lo, hi = (0, 8) if half == 0 else (8, 15)
                for dr in range(lo, hi):
                    col = (dr - lo) * 64
                    S.op("pe", lambda e: e.matmul(pb_ap[:, col:col + 64], tpp[:, dr, :], c["J"][:], start=True, stop=True),
                         reads=[btpp, c["buf"]], writes=[pbb[col // 512]])
                for dr in range(lo, hi):
                    col = (dr - lo) * 64
                    S.op("dve", lambda e: e.tensor_tensor(out=Tm[:, dr, :], in0=pb_ap[:, col:col + 64], in1=c["mneg"][:],
                                                          op=ALU.add), reads=[pbb[col // 512], c["buf"]], writes=[bTm])
            pats = {}
            for t in range(NT):
                Q = qT[iq % 2]
                bQ = bq[iq % 2]
                S.dma("sp", Q[:], qTb[h, :, t * 512:(t + 1) * 512], writes=[bQ])
                for bi in range(4):
                    b = t * 4 + bi
                    r0, r1 = 2 * b, 2 * b + 1
                    a0 = rs(r0) // 2
                    a1 = (rs(r1) + 7) // 2
                    chunks = list(range(a0, a1 + 1))
                    key = tuple((kap - r if rs(r) <= kap < rs(r) + 8 else None)
                                for a in chunks for kap in (2 * a, 2 * a + 1) for r in (r0, r1))
                    if key not in pats:
                        pi = len(pats)
                        assert pi < NPAT
                        pats[key] = pi
                        ki = 0
                        for ai, a in enumerate(chunks):
                            for kh in range(2):
                                for rh in range(2):
                                    d = key[ki]
                                    ki += 1
                                    dst = Bt[pi][64 * kh:64 * kh + 64, ai, 64 * rh:64 * rh + 64]
                                    if d is None:
                                        S.op("dve", lambda e: e.memset(dst, NEG), writes=[bBt[pi]])
                                    else:
                                        S.op("dve", lambda e: e.tensor_copy(out=dst, in_=Tm[64 * kh:64 * kh + 64, d + 7, :]),
                                             reads=[bTm], writes=[bBt[pi]])
                    pi = pats[key]
                    nch = len(chunks)
                    i2 = ib % 2
                    ib += 1
                    ps = kb.pq[i2]
                    bps = [kb.pb[2 * i2], kb.pb[2 * i2 + 1]]
                    for ai, a in enumerate(chunks):
                        S.op("pe", lambda e: e.matmul(ps[:, ai * 128:(ai + 1) * 128], kT[:, a * 128:(a + 1) * 128],
                                                      Q[:, bi * 128:(bi + 1) * 128], start=True, stop=True),
                             reads=[bkT, bQ], writes=[bps[ai // 4]])
                    S.op("dve", lambda e: e.scalar_tensor_tensor(
                        out=sc[i2][:, 0:nch * 128], in0=ps[:, 0:nch * 128], scalar=scale,
                        in1=Bt[pi][:, 0:nch, :].rearrange("p a q -> p (a q)"), op0=ALU.mult, op1=ALU.add),
                        reads=bps + [bBt[pi]], writes=[bsc[i2]])
                    S.op("act", lambda e: e.activation(out=pT[i2][:, 0:nch * 128], in_=sc[i2][:, 0:nch * 128], func=AF.Exp),
                         reads=[bsc[i2]], writes=[bpT[i2]])
                    for ai, a in enumerate(chunks):
                        S.op("pe", lambda e: e.matmul(OT[:, bi * 128:(bi + 1) * 128], v[:, a, :], pT[i2][:, ai * 128:(ai + 1) * 128],
                                                      start=(ai == 0), stop=(ai == nch - 1)),
                             reads=[bv, bpT[i2]], writes=[bOT])
                    for ai, a in enumerate(chunks):
                        S.op("pe", lambda e: e.matmul(MS[:, bi * 128:(bi + 1) * 128], c["ones_b"][:], pT[i2][:, ai * 128:(ai + 1) * 128],
                                                      start=(ai == 0), stop=(ai == nch - 1)),
                             reads=[bpT[i2], c["buf"]], writes=[bMS])
                S.op("dve", lambda e: e.reciprocal(out=rsum[:], in_=MS), reads=[bMS], writes=[brs])
                OB = ob[iq % 2]
                bOB = bob[iq % 2]
                S.op("dve", lambda e: e.tensor_tensor(out=OB[:], in0=OT, in1=rsum[:], op=ALU.mult),
                     reads=[bOT, brs], writes=[bOB])
                S.dma("sp", oT[1024 + h * 128:1024 + (h + 1) * 128, t * 512:(t + 1) * 512], OB[:], reads=[bOB], writes=[boT])
                iq += 1
    S.barrier()


def layer_norm_tm(kb, z, bz, st, mv, rstd, bst, gt, bt, bgb):
    S = kb.S
    for i in range(4):
        S.op("dve", lambda e: e.bn_stats(out=st[:, i, :], in_=z[:, i * 512:(i + 1) * 512]), reads=[bz], writes=[bst])
    S.op("dve", lambda e: e.bn_aggr(out=mv[:], in_=st[:]), reads=[bst], writes=[bst])
    S.op("dve", lambda e: e.tensor_scalar(out=rstd[:], in0=mv[:, 1:2], scalar1=LN_EPS, scalar2=None, op0=ALU.add),
         reads=[bst], writes=[bst])
    S.op("act", lambda e: e.activation(out=rstd[:], in_=rstd[:], func=AF.Sqrt), reads=[bst], writes=[bst])
    S.op("dve", lambda e: e.reciprocal(out=rstd[:], in_=rstd[:]), reads=[bst], writes=[bst])
    S.op("dve", lambda e: e.tensor_scalar(out=z[:], in0=z[:], scalar1=mv[:, 0:1], scalar2=rstd[:, 0:1],
                                          op0=ALU.subtract, op1=ALU.mult), reads=[bst, bz], writes=[bz])
    S.op("dve", lambda e: e.tensor_tensor(out=z[:], in0=z[:], in1=gt[:], op=ALU.mult), reads=[bz, bgb], writes=[bz])
    S.op("dve", lambda e: e.tensor_tensor(out=z[:], in0=z[:], in1=bt[:], op=ALU.add), reads=[bz, bgb], writes=[bz])


def load_bcast(kb, dst, bdst, src_row):
    ap = bass.AP(src_row.tensor, src_row.offset, [[0, 128]] + [list(x) for x in src_row.ap])
    kb.S.dma("sp", dst, ap, writes=[bdst])


def s45_post(kb, c, job, layer, P):
    nc, S = kb.nc, kb.S
    n = job["name"]
    Sn = job["S"]
    C = job["cap"]
    Cs = C + 128
    NT = Sn // 512
    oT = kb.dr[f"oT_{n}"]
    boT = kb.db[f"oT_{n}"]
    w_out = kb.dr["ev_w_out" if layer == 0 else "od_w_out"]
    xres = kb.dr[f"x_{n}"] if layer == 0 else kb.dr[f"x1_{n}"]
    bxres = kb.db[f"x_{n}"] if layer == 0 else kb.db[f"x1_{n}"]
    xmid = kb.dr[f"xmid_{n}"]
    bxmid = kb.db[f"xmid_{n}"]
    xbuf = kb.dr[f"xbuf_{n}"]
    bxbuf = kb.db[f"xbuf_{n}"]
    S.barrier()
    with ExitStack() as es:
        def sb(name, shape, dt):
            return es.enter_context(nc.sbuf_tensor(U(name), shape, dt))
        Wo = sb("s4wo", [128, 16, 2048], BF16); bWo = Buf()
        load_w_bf16(kb, Wo, bWo, w_out, ksplit=8)
        gt = sb("s4g", [128, 2048], F32)
        bt = sb("s4b", [128, 2048], F32)
        bgb = Buf()
        load_bcast(kb, gt[:], bgb, kb.dr["ln1_g"][layer])
        load_bcast(kb, bt[:], bgb, kb.dr["ln1_b"][layer])
        Wge = sb("s4wge", [128, 16, 36], F32); bWge = Buf()
        S.dma("sp", Wge[:, :, 0:4], kb.dr["moe_w_group"][layer].rearrange("(kc p) g -> p kc g", p=128), writes=[bWge])
        S.dma("sp", Wge[:, :, 4:36], kb.dr["moe_w_expert"][layer].rearrange("(kc p) g -> p kc g", p=128), writes=[bWge])
        bias = sb("s4bias", [128, 36], F32); bbias = Buf()
        load_bcast(kb, bias[:, 0:4], bbias, kb.dr["moe_b_group"][layer])
        load_bcast(kb, bias[:, 4:36], bbias, kb.dr["moe_b_expert"][layer])
        eC = sb("s4eC", [128, 32], F32); beC = Buf()
        S.dma("sp", eC[:], kb.dr[f"ecs_{n}"][:, :], writes=[beC])
        cnt = sb("s4cnt", [1, 32], F32); bcnt = Buf()
        S.op("dve", lambda e: e.memset(cnt[:], 0.0), writes=[bcnt])
        zt = sb("s4zero", [128, 2048], BF16); bzt = Buf()
        S.op("dve", lambda e: e.memset(zt[:], 0.0), writes=[bzt])
        for r in range(0, 32 * Cs, 128):
            S.dma("sp", xbuf[r:r + 128, :], zt[:], reads=[bzt], writes=[bxbuf])
        ybuf_ = kb.dr[f"ybuf_{n}"]
        for e_ in range(32):
            S.dma("sp", ybuf_[e_ * Cs + C:(e_ + 1) * Cs, :], zt[:], reads=[bzt], writes=[kb.db[f"ybuf_{n}"]])
        ot = [sb(f"s4ot{i}", [128, 16, 512], BF16) for i in range(2)]
        bot = [Buf(), Buf()]
        xt = [sb(f"s4x{i}", [128, 2048], F32) for i in range(2)]
        bxt = [Buf(), Buf()]
        z = [sb(f"s4z{i}", [128, 2048], F32) for i in range(2)]
        bz = [Buf(), Buf()]
        st = sb("s4st", [128, 4, 6], F32)
        mv = sb("s4mv", [128, 2], F32)
        rstd = sb("s4rstd", [128, 1], F32)
        bst = Buf()
        xmT = sb("s4xmT", [128, 16, 128], F32); bxmT = Buf()
        lg = sb("s4lg", [128, 36], F32); blg = Buf()
        r_ = {}
        for nm, shp in (("gmax", [128, 1]), ("gsel", [128, 4]), ("gex", [128, 4]), ("gsum", [128, 1]), ("pg", [128, 1]),
                        ("em", [128, 4, 8]), ("ein", [128, 8]), ("m1", [128, 1]), ("oh1", [128, 8]), ("ein2", [128, 8]),
                        ("m2", [128, 1]), ("oh2", [128, 8]), ("dd", [128, 1]), ("w1", [128, 1]), ("w2", [128, 1]),
                        ("M1", [128, 4, 8]), ("M2", [128, 4, 8]), ("M", [128, 32]), ("rk", [128, 32]), ("pr", [128, 32]),
                        ("sl", [128, 2])):
            r_[nm] = sb(f"s4r_{nm}", shp, F32)
        br = Buf()
        it = 0
        for T in range(NT):
            O = ot[T % 2]
            bO = bot[T % 2]
            src = oT[:, T * 512:(T + 1) * 512].rearrange("(kc p) t -> p kc t", p=128)
            for k0 in range(0, 16, 4):
                S.dma("sp", O[:, k0:k0 + 4, :], src[:, k0:k0 + 4, :], reads=[boT], writes=[bO])
            for sub in range(4):
                t = T * 4 + sub
                X = xt[it % 2]
                bX = bxt[it % 2]
                Z = z[it % 2]
                bZ = bz[it % 2]
                it += 1
                S.dma("sp", X[:], xres[t * 128:(t + 1) * 128, :], reads=[bxres], writes=[bX])
                for cg in range(4):
                    A, bA = kb.bank((sub % 2) * 4 + cg)
                    for kc in range(16):
                        S.op("pe", lambda e: e.matmul(A, O[:, kc, sub * 128:(sub + 1) * 128], Wo[:, kc, cg * 512:(cg + 1) * 512],
                                                      start=(kc == 0), stop=(kc == 15)), reads=[bO, bWo], writes=[bA])
                    S.op("dve", lambda e: e.scalar_tensor_tensor(out=Z[:, cg * 512:(cg + 1) * 512], in0=X[:, cg * 512:(cg + 1) * 512],
                                                                 scalar=DN_ALPHA, in1=A, op0=ALU.mult, op1=ALU.add),
                         reads=[bX, bA], writes=[bZ])
                layer_norm_tm(kb, Z, bZ, st, mv, rstd, bst, gt, bt, bgb)
                S.dma("sp", xmid[t * 128:(t + 1) * 128, :], Z[:], reads=[bZ], writes=[bxmid])
                if job.get("noroute"):
                    continue
                for q4 in range(4):
                    A, bA = kb.bank(((sub + 1) % 2) * 4 + q4)
                    for j in range(4):
                        kc = q4 * 4 + j
                        S.op("pe", lambda e: e.transpose(A[:, j * 128:(j + 1) * 128], Z[:, kc * 128:(kc + 1) * 128], c["ident"][:]),
                             reads=[bZ, c["buf"]], writes=[bA])
                    S.op("act", lambda e: e.activation(out=xmT[:, q4 * 4:(q4 + 1) * 4, :].rearrange("p a t -> p (a t)"), in_=A,
                                                       func=AF.Copy), reads=[bA], writes=[bxmT])
                A, bA = kb.bank(((sub + 1) % 2) * 4)
                for kc in range(16):
                    S.op("pe", lambda e: e.matmul(A[:, 0:36], xmT[:, kc, :], Wge[:, kc, :], start=(kc == 0), stop=(kc == 15)),
                         reads=[bxmT, bWge], writes=[bA])
                S.op("dve", lambda e: e.tensor_tensor(out=lg[:], in0=A[:, 0:36], in1=bias[:], op=ALU.add),
                     reads=[bA, bbias], writes=[blg])
                route_tile(kb, c, S, r_, br, lg, blg, cnt, bcnt, eC, beC, C, P, t, ((sub + 1) % 2) * 4 + 1)
                for k in range(2):
                    S.idma(xbuf[:, :], bass.IndirectOffsetOnAxis(ap=P["idx"][:, t, k:k + 1], axis=0), Z[:], None,
                           reads=[bZ, P["bidx"]], writes=[bxbuf])
    S.barrier()


def route_tile(kb, c, S, r_, br, lg, blg, cnt, bcnt, eC, beC, C, P, t, bank_i):
    dv = lambda fn, reads, writes: S.op("dve", fn, reads=reads, writes=writes)
    R = [br, blg]
    gl = lg[:, 0:4]
    dv(lambda e: e.tensor_reduce(out=r_["gmax"][:], in_=gl, axis=AX.X, op=ALU.max), R, [br])
    dv(lambda e: e.tensor_scalar(out=r_["gsel"][:], in0=gl, scalar1=r_["gmax"][:, 0:1], scalar2=None, op0=ALU.is_ge), R, [br])
    dv(lambda e: e.tensor_scalar(out=r_["gex"][:], in0=gl, scalar1=r_["gmax"][:, 0:1], scalar2=None, op0=ALU.subtract), R, [br])
    S.op("act", lambda e: e.activation(out=r_["gex"][:], in_=r_["gex"][:], func=AF.Exp), reads=[br], writes=[br])
    dv(lambda e: e.tensor_reduce(out=r_["gsum"][:], in_=r_["gex"][:], axis=AX.X, op=ALU.add), R, [br])
    dv(lambda e: e.reciprocal(out=r_["pg"][:], in_=r_["gsum"][:]), R, [br])
    el = lg[:, 4:36].rearrange("p (g j) -> p g j", j=8)
    for g in range(4):
        dv(lambda e: e.tensor_scalar(out=r_["em"][:, g, :], in0=el[:, g, :], scalar1=r_["gsel"][:, g:g + 1], scalar2=None,
                                     op0=ALU.mult), R, [br])
    dv(lambda e: e.tensor_tensor(out=r_["ein"][:], in0=r_["em"][:, 0, :], in1=r_["em"][:, 1, :], op=ALU.add), R, [br])
    dv(lambda e: e.tensor_tensor(out=r_["ein"][:], in0=r_["ein"][:], in1=r_["em"][:, 2, :], op=ALU.add), R, [br])
    dv(lambda e: e.tensor_tensor(out=r_["ein"][:], in0=r_["ein"][:], in1=r_["em"][:, 3, :], op=ALU.add), R, [br])
    dv(lambda e: e.tensor_reduce(out=r_["m1"][:], in_=r_["ein"][:], axis=AX.X, op=ALU.max), R, [br])
    dv(lambda e: e.tensor_scalar(out=r_["oh1"][:], in0=r_["ein"][:], scalar1=r_["m1"][:, 0:1], scalar2=None, op0=ALU.is_ge), R, [br])
    dv(lambda e: e.scalar_tensor_tensor(out=r_["ein2"][:], in0=r_["oh1"][:], scalar=-1e30, in1=r_["ein"][:],
                                        op0=ALU.mult, op1=ALU.add), R, [br])
    dv(lambda e: e.tensor_reduce(out=r_["m2"][:], in_=r_["ein2"][:], axis=AX.X, op=ALU.max), R, [br])
    dv(lambda e: e.tensor_scalar(out=r_["oh2"][:], in0=r_["ein2"][:], scalar1=r_["m2"][:, 0:1], scalar2=None, op0=ALU.is_ge), R, [br])
    dv(lambda e: e.tensor_tensor(out=r_["dd"][:], in0=r_["m2"][:], in1=r_["m1"][:], op=ALU.subtract), R, [br])
    S.op("act", lambda e: e.activation(out=r_["dd"][:], in_=r_["dd"][:], func=AF.Exp), reads=[br], writes=[br])
    dv(lambda e: e.tensor_scalar(out=r_["dd"][:], in0=r_["dd"][:], scalar1=1.0, scalar2=None, op0=ALU.add), R, [br])
    dv(lambda e: e.reciprocal(out=r_["w1"][:], in_=r_["dd"][:]), R, [br])
    dv(lambda e: e.tensor_scalar(out=r_["w2"][:], in0=r_["w1"][:], scalar1=-1.0, scalar2=1.0, op0=ALU.mult, op1=ALU.add), R, [br])
    dv(lambda e: e.tensor_tensor(out=P["gate"][:, t, 0:1], in0=r_["w1"][:], in1=r_["pg"][:], op=ALU.mult), R, [br, P["bgate"]])
    dv(lambda e: e.tensor_tensor(out=P["gate"][:, t, 1:2], in0=r_["w2"][:], in1=r_["pg"][:], op=ALU.mult), R, [br, P["bgate"]])
    for g in range(4):
        dv(lambda e: e.tensor_scalar(out=r_["M1"][:, g, :], in0=r_["oh1"][:], scalar1=r_["gsel"][:, g:g + 1], scalar2=None,
                                     op0=ALU.mult), R, [br])
        dv(lambda e: e.tensor_scalar(out=r_["M2"][:, g, :], in0=r_["oh2"][:], scalar1=r_["gsel"][:, g:g + 1], scalar2=None,
                                     op0=ALU.mult), R, [br])
    M1f = r_["M1"][:].rearrange("p g j -> p (g j)")
    M2f = r_["M2"][:].rearrange("p g j -> p (g j)")
    dv(lambda e: e.tensor_tensor(out=r_["M"][:], in0=M1f, in1=M2f, op=ALU.add), R, [br])
    A, bA = kb.bank(bank_i)
    S.op("pe", lambda e: e.matmul(A[:, 0:32], c["U"][:], r_["M"][:], start=True, stop=False), reads=[br, c["buf"]], writes=[bA])
    S.op("pe", lambda e: e.matmul(A[:, 0:32], c["ones_f"][0:1, :], cnt[0:1, :], start=False, stop=True),
         reads=[bcnt, c["buf"]], writes=[bA])
    dv(lambda e: e.tensor_scalar(out=r_["rk"][:], in0=A[:, 0:32], scalar1=float(C), scalar2=None, op0=ALU.min), R + [bA], [br])
    dv(lambda e: e.tensor_tensor(out=r_["rk"][:], in0=r_["rk"][:], in1=eC[:], op=ALU.add), R + [beC], [br])
    B2, bB2 = kb.bank(bank_i + 1)
    S.op("pe", lambda e: e.matmul(B2[0:1, 0:32], c["ones_f"][:, 0:1], r_["M"][:], start=True, stop=True),
         reads=[br, c["buf"]], writes=[bB2])
    dv(lambda e: e.tensor_tensor(out=cnt[:], in0=cnt[:], in1=B2[0:1, 0:32], op=ALU.add), [bB2, bcnt], [bcnt])
    for k, Mk in enumerate((M1f, M2f)):
        dv(lambda e: e.tensor_tensor(out=r_["pr"][:], in0=r_["rk"][:], in1=Mk, op=ALU.mult), R, [br])
        dv(lambda e: e.tensor_reduce(out=r_["sl"][:, k:k + 1], in_=r_["pr"][:], axis=AX.X, op=ALU.add), R, [br])
    dv(lambda e: e.tensor_copy(out=P["idx"][:, t, :], in_=r_["sl"][:]), R, [br, P["bidx"]])


def s6_experts(kb, c, job, layer):
    nc, S = kb.nc, kb.S
    n = job["name"]
    C = job["cap"]
    Cs = C + 128
    TS = min(512, C)
    xbuf = kb.dr[f"xbuf_{n}"]
    bxbuf = kb.db[f"xbuf_{n}"]
    ybuf = kb.dr[f"ybuf_{n}"]
    bybuf = kb.db[f"ybuf_{n}"]
    w1, w3, w2 = kb.dr["moe_w1"], kb.dr["moe_w3"], kb.dr["moe_w2"]
    S.barrier()
    with ExitStack() as es:
        def sb(name, shape, dt):
            return es.enter_context(nc.sbuf_tensor(U(name), shape, dt))
        W1 = [sb(f"s6w1{i}", [128, 16, 512], BF16) for i in range(2)]
        W3 = [sb(f"s6w3{i}", [128, 16, 512], BF16) for i in range(2)]
        W2 = [sb(f"s6w2{i}", [128, 4, 2048], BF16) for i in range(2)]
        bW = [Buf(), Buf()]
        xr = [sb(f"s6xr{i}", [128, 2048], F32) for i in range(2)]
        bxr = [Buf(), Buf()]
        xeT = sb("s6xeT", [128, 16, TS], BF16); bxeT = Buf()
        sl = sb("s6sl", [128, TS], F32); bsl = Buf()
        hm = sb("s6hm", [128, 4, TS], BF16); bhm = Buf()
        yst = [sb(f"s6y{i}", [128, 2048], F32) for i in range(2)]
        byst = [Buf(), Buf()]

        def loadw(e, i):
            load_w_bf16(kb, W1[i], bW[i], w1[layer, e], ksplit=4)
            load_w_bf16(kb, W3[i], bW[i], w3[layer, e], ksplit=4)
            load_w_bf16(kb, W2[i], bW[i], w2[layer, e], ksplit=4)
        loadw(0, 0)
        ix = 0
        iy = 0
        ib = 0
        for e in range(job.get("nexp", 32)):
            if e + 1 < 32:
                loadw(e + 1, (e + 1) % 2)
            i = e % 2
            for s0 in range(0, C, TS):
                nsub = TS // 128
                for sub in range(nsub):
                    XR = xr[ix % 2]
                    bXR = bxr[ix % 2]
                    ix += 1
                    r0 = e * Cs + s0 + sub * 128
                    S.dma("pool", XR[:], xbuf[r0:r0 + 128, :], reads=[bxbuf], writes=[bXR])
                    for q4 in range(4):
                        A, bA = kb.bank(ib % 8)
                        ib += 1
                        for j in range(4):
                            kc = q4 * 4 + j
                            S.op("pe", lambda e_: e_.transpose(A[:, j * 128:(j + 1) * 128], XR[:, kc * 128:(kc + 1) * 128], c["ident"][:]),
                                 reads=[bXR, c["buf"]], writes=[bA])
                        dstv = xeT[:, q4 * 4:(q4 + 1) * 4, sub * 128:(sub + 1) * 128]
                        srcv = A.rearrange("p (a t) -> p a t", t=128)
                        if q4 % 2 == 0:
                            S.op("act", lambda e_: e_.activation(out=dstv, in_=srcv, func=AF.Copy), reads=[bA], writes=[bxeT])
                        else:
                            S.op("dve", lambda e_: e_.tensor_copy(out=dstv, in_=srcv), reads=[bA], writes=[bxeT])
                for c4 in range(4):
                    A, bA = kb.bank(ib % 8)
                    ib += 1
                    B, bB = kb.bank(ib % 8)
                    ib += 1
                    for kc in range(16):
                        S.op("pe", lambda e_: e_.matmul(A[:, 0:TS], W1[i][:, kc, c4 * 128:(c4 + 1) * 128], xeT[:, kc, :],
                                                        start=(kc == 0), stop=(kc == 15)), reads=[bW[i], bxeT], writes=[bA])
                    for kc in range(16):
                        S.op("pe", lambda e_: e_.matmul(B[:, 0:TS], W3[i][:, kc, c4 * 128:(c4 + 1) * 128], xeT[:, kc, :],
                                                        start=(kc == 0), stop=(kc == 15)), reads=[bW[i], bxeT], writes=[bB])
                    S.op("act", lambda e_: e_.activation(out=sl[:], in_=A[:, 0:TS], func=AF.Silu), reads=[bA], writes=[bsl])
                    S.op("dve", lambda e_: e_.tensor_tensor(out=hm[:, c4, :], in0=sl[:], in1=B[:, 0:TS], op=ALU.mult),
                         reads=[bsl, bB], writes=[bhm])
                for sub in range(nsub):
                    Y = yst[iy % 2]
                    bY = byst[iy % 2]
                    iy += 1
                    for cg in range(4):
                        A, bA = kb.bank(ib % 8)
                        ib += 1
                        for k4 in range(4):
                            S.op("pe", lambda e_: e_.matmul(A, hm[:, k4, sub * 128:(sub + 1) * 128], W2[i][:, k4, cg * 512:(cg + 1) * 512],
                                                            start=(k4 == 0), stop=(k4 == 3)), reads=[bhm, bW[i]], writes=[bA])
                        if cg % 2 == 0:
                            S.op("act", lambda e_: e_.activation(out=Y[:, cg * 512:(cg + 1) * 512], in_=A, func=AF.Copy),
                                 reads=[bA], writes=[bY])
                        else:
                            S.op("dve", lambda e_: e_.tensor_copy(out=Y[:, cg * 512:(cg + 1) * 512], in_=A), reads=[bA], writes=[bY])
                    r0 = e * Cs + s0 + sub * 128
                    S.dma("pool", ybuf[r0:r0 + 128, :], Y[:], reads=[bY], writes=[bybuf])
    S.barrier()


def s7_combine(kb, c, job, layer, P):
    nc, S = kb.nc, kb.S
    n = job["name"]
    Sn = job["S"]
    NTt = Sn // 128
    xmid = kb.dr[f"xmid_{n}"]
    bxmid = kb.db[f"xmid_{n}"]
    ybuf = kb.dr[f"ybuf_{n}"]
    bybuf = kb.db[f"ybuf_{n}"]
    last = (layer == DEPTH - 1)
    if last:
        dst, bdst = kb.dr[f"y_{n}"], kb.db[f"y_{n}"]
    else:
        dst, bdst = kb.dr[f"x1_{n}"], kb.db[f"x1_{n}"]
        x1T, bx1T = kb.dr[f"x1T_{n}"], kb.db[f"x1T_{n}"]
    S.barrier()
    with ExitStack() as es:
        def sb(name, shape, dt):
            return es.enter_context(nc.sbuf_tensor(U(name), shape, dt))
        gt = sb("s7g", [128, 2048], F32)
        bt = sb("s7b", [128, 2048], F32)
        bgb = Buf()
        load_bcast(kb, gt[:], bgb, kb.dr["ln2_g"][layer])
        load_bcast(kb, bt[:], bgb, kb.dr["ln2_b"][layer])
        xm = [sb(f"s7xm{i}", [128, 2048], F32) for i in range(2)]
        bxm = [Buf(), Buf()]
        r1 = [sb(f"s7r1{i}", [128, 2048], F32) for i in range(2)]
        r2 = [sb(f"s7r2{i}", [128, 2048], F32) for i in range(2)]
        brr = [Buf(), Buf()]
        st = sb("s7st", [128, 4, 6], F32)
        mv = sb("s7mv", [128, 2], F32)
        rstd = sb("s7rstd", [128, 1], F32)
        bst = Buf()
        xTs = [sb(f"s7xT{i}", [128, 16, 128], BF16) for i in range(2)]
        bxTs = [Buf(), Buf()]
        ib = 0
        for t in range(NTt):
            i = t % 2
            Z = xm[i]
            bZ = bxm[i]
            S.dma("sp", Z[:], xmid[t * 128:(t + 1) * 128, :], reads=[bxmid], writes=[bZ])
            S.idma(r1[i][:], None, ybuf[:, :], bass.IndirectOffsetOnAxis(ap=P["idx"][:, t, 0:1], axis=0),
                   reads=[bybuf, P["bidx"]], writes=[brr[i]])
            S.idma(r2[i][:], None, ybuf[:, :], bass.IndirectOffsetOnAxis(ap=P["idx"][:, t, 1:2], axis=0),
                   reads=[bybuf, P["bidx"]], writes=[brr[i]])
            S.op("dve", lambda e: e.tensor_scalar(out=Z[:], in0=Z[:], scalar1=DN_ALPHA, scalar2=None, op0=ALU.mult),
                 reads=[bZ], writes=[bZ])
            S.op("dve", lambda e: e.scalar_tensor_tensor(out=Z[:], in0=r1[i][:], scalar=P["gate"][:, t, 0:1], in1=Z[:],
                                                         op0=ALU.mult, op1=ALU.add), reads=[bZ, brr[i], P["bgate"]], writes=[bZ])
            S.op("dve", lambda e: e.scalar_tensor_tensor(out=Z[:], in0=r2[i][:], scalar=P["gate"][:, t, 1:2], in1=Z[:],
                                                         op0=ALU.mult, op1=ALU.add), reads=[bZ, brr[i], P["bgate"]], writes=[bZ])
            layer_norm_tm(kb, Z, bZ, st, mv, rstd, bst, gt, bt, bgb)
            S.dma("sp", dst[t * 128:(t + 1) * 128, :], Z[:], reads=[bZ], writes=[bdst])
            if not last:
                XT = xTs[i]
                bXT = bxTs[i]
                for q4 in range(4):
                    A, bA = kb.bank(ib % 8)
                    ib += 1
                    for j in range(4):
                        kc = q4 * 4 + j
                        S.op("pe", lambda e: e.transpose(A[:, j * 128:(j + 1) * 128], Z[:, kc * 128:(kc + 1) * 128], c["ident"][:]),
                             reads=[bZ, c["buf"]], writes=[bA])
                    S.op("act", lambda e: e.activation(out=XT[:, q4 * 4:(q4 + 1) * 4, :].rearrange("p a t -> p (a t)"), in_=A,
                                                       func=AF.Copy), reads=[bA], writes=[bXT])
                xv = x1T[:, t * 128:(t + 1) * 128].rearrange("(kc p) t -> p kc t", p=128)
                for k0 in (0, 8):
                    S.dma("sp", xv[:, k0:k0 + 8, :], XT[:, k0:k0 + 8, :], reads=[bXT], writes=[bx1T])
    S.barrier()


def s1_l1(kb, c, job):
    nc, S = kb.nc, kb.S
    n = job["name"]
    Sn = job["S"]
    NT = Sn // 512
    x1T = kb.dr[f"x1T_{n}"]
    bx1T = kb.db[f"x1T_{n}"]
    d = lambda k: (kb.dr[f"{k}_{n}"], kb.db[f"{k}_{n}"])
    qnT, bqnT = d("qnT"); knT, bknT = d("knT"); gqT, bgqT = d("gqT"); qpeT, bqpeT = d("qpeT")
    kpeT, bkpeT = d("kpeT"); gkT, bgkT = d("gkT"); vm, bvm = d("vm"); gv, bgv = d("gv")
    S.barrier()
    with ExitStack() as es:
        def sb(name, shape, dt):
            return es.enter_context(nc.sbuf_tensor(U(name), shape, dt))
        Win = sb("l1win", [128, 16, 2368], BF16); bWin = Buf()
        wi = kb.dr["od_w_in"].rearrange("(kc p) n -> p kc n", p=128)
        for k0 in range(16):
            for c0 in (0, 1184):
                S.dma("pool", Win[:, k0:k0 + 1, c0:c0 + 1184], wi[:, k0:k0 + 1, c0:c0 + 1184], writes=[bWin])
        Wq = sb("l1wq", [128, 4, 1536], BF16); bWq = Buf()
        load_w_bf16(kb, Wq, bWq, kb.dr["mla_w_q_up"], ksplit=4)
        Wkv = sb("l1wkv", [128, 2, 2048], BF16); bWkv = Buf()
        load_w_bf16(kb, Wkv, bWkv, kb.dr["mla_w_kv_up"], ksplit=2)
        ncol = sb("l1ncol", [128, 8], F32); bncol = Buf()

        def colload(j, src1d, off):
            ap = bass.AP(src1d.tensor, src1d.offset + off, [[1, 128], [1, 1]])
            S.dma("sp", ncol[:, j:j + 1], ap, writes=[bncol])
        for j in range(4):
            colload(j, kb.dr["mla_q_norm"][0], j * 128)
        for j in range(2):
            colload(4 + j, kb.dr["mla_kv_norm"][0], j * 128)
        colload(6, kb.dr["gqa_q_norm"][0], 0)
        colload(7, kb.dr["gqa_k_norm"][0], 0)
        X = [sb(f"l1x{i}", [128, 16, 512], BF16) for i in range(2)]
        bX = [Buf(), Buf()]
        tabs = {}
        for nm, pp in (("M_c", 64), ("M_s", 64), ("G_c", 128), ("G_s", 128)):
            tabs[nm] = [sb(f"l1t{nm}{i}", [pp, 512], F32) for i in range(2)]
        btab = [Buf(), Buf()]
        a32 = sb("l1a32", [128, 4, 512], F32); ba32 = Buf()
        sqb = sb("l1sqb", [128, 512], BF16); bsqb = Buf()
        rstd = sb("l1rstd", [128, 512], F32); brstd = Buf()
        qn = sb("l1qn", [128, 4, 512], BF16); bqn = Buf()
        kvn = sb("l1kvn", [128, 2, 512], BF16); bkvn = Buf()
        g32 = sb("l1g32", [128, 512], F32); bg32 = Buf()
        qa = sb("l1qa", [128, 512], BF16); bqa = Buf()
        t1 = sb("l1t1", [128, 512], F32); bt1 = Buf()
        t2 = sb("l1t2", [128, 512], F32); bt2 = Buf()
        og = [sb(f"l1og{i}", [128, 512], BF16) for i in range(3)]
        bog = [Buf() for _ in range(3)]
        vs = [sb(f"l1vs{i}", [128, 1024], BF16) for i in range(2)]
        bvs = [Buf(), Buf()]
        st = {"ib": 0, "io": 0, "iv": 0}
        MS, bMS = kb.bank(7)
        RB, bRB = kb.bank(6)

        def nbank():
            r = kb.bank(st["ib"] % 6)
            st["ib"] += 1
            return r

        def nog():
            i = st["io"] % 3
            st["io"] += 1
            return og[i], bog[i]

        def rms_chunks(cols0, nchunk, ncol0, dim, dst, bdst, Xt, bXt):
            for cc in range(nchunk):
                A, bA = nbank()
                for kc in range(16):
                    S.op("pe", lambda e: e.matmul(A, Win[:, kc, cols0 + cc * 128:cols0 + (cc + 1) * 128], Xt[:, kc, :],
                                                  start=(kc == 0), stop=(kc == 15)), reads=[bWin, bXt], writes=[bA])
                S.op("act", lambda e: e.activation(out=a32[:, cc, :], in_=A, func=AF.Copy), reads=[bA], writes=[ba32])
                S.op("act", lambda e: e.activation(out=sqb[:], in_=a32[:, cc, :], func=AF.Square), reads=[ba32], writes=[bsqb])
                S.op("pe", lambda e: e.matmul(MS, c["ones_b"][:], sqb[:], start=(cc == 0), stop=(cc == nchunk - 1)),
                     reads=[bsqb, c["buf"]], writes=[bMS])
            S.op("dve", lambda e: e.tensor_scalar(out=rstd[:], in0=MS, scalar1=1.0 / dim, scalar2=RMS_EPS,
                                                  op0=ALU.mult, op1=ALU.add), reads=[bMS], writes=[brstd])
            S.op("act", lambda e: e.activation(out=rstd[:], in_=rstd[:], func=AF.Sqrt), reads=[brstd], writes=[brstd])
            S.op("dve", lambda e: e.reciprocal(out=rstd[:], in_=rstd[:]), reads=[brstd], writes=[brstd])
            for cc in range(nchunk):
                S.op("dve", lambda e: e.scalar_tensor_tensor(out=dst[:, cc, :], in0=a32[:, cc, :],
                                                             scalar=ncol[:, ncol0 + cc:ncol0 + cc + 1], in1=rstd[:],
                                                             op0=ALU.mult, op1=ALU.mult),
                     reads=[ba32, brstd, bncol], writes=[bdst])

        for t in range(NT):
            Xt = X[t % 2]
            bXt = bX[t % 2]
            src = x1T[:, t * 512:(t + 1) * 512].rearrange("(kc p) n -> p kc n", p=128)
            for k0 in range(0, 16, 4):
                S.dma("sp", Xt[:, k0:k0 + 4, :], src[:, k0:k0 + 4, :], reads=[bx1T], writes=[bXt])
            bT = btab[t % 2]
            tb = {k: v[t % 2] for k, v in tabs.items()}
            for nm in ("M_c", "M_s", "G_c", "G_s"):
                S.dma("sp", tb[nm][:], kb.dr[f"rope{nm}"][:, t * 512:(t + 1) * 512], writes=[bT])
            cs = slice(t * 512, (t + 1) * 512)
            rms_chunks(0, 4, 0, 512, qn, bqn, Xt, bXt)
            for h in range(8):
                A, bA = nbank()
                for cc in range(4):
                    S.op("pe", lambda e: e.matmul(A, Wq[:, cc, h * 192:h * 192 + 128], qn[:, cc, :], start=(cc == 0), stop=(cc == 3)),
                         reads=[bWq, bqn], writes=[bA])
                O, bO = nog()
                S.op("act", lambda e: e.activation(out=O[:], in_=A, func=AF.Copy), reads=[bA], writes=[bO])
                S.dma("sp", qnT[h, :, cs], O[:], reads=[bO], writes=[bqnT])
                A, bA = nbank()
                for cc in range(4):
                    S.op("pe", lambda e: e.matmul(A[0:64, :], Wq[:, cc, h * 192 + 128:h * 192 + 192], qn[:, cc, :],
                                                  start=(cc == 0), stop=(cc == 3)), reads=[bWq, bqn], writes=[bA])
                O, bO = nog()
                rope_evac(kb, c, A, bA, RB, bRB, c["PM"], 64, tb["M_c"], tb["M_s"], bT, qa, bqa, t1, bt1, t2, bt2, O, bO)
                S.dma("sp", qpeT[h, :, cs], O[0:64, :], reads=[bO], writes=[bqpeT])
            rms_chunks(512, 2, 4, 256, kvn, bkvn, Xt, bXt)
            for h in range(8):
                A, bA = nbank()
                for cc in range(2):
                    S.op("pe", lambda e: e.matmul(A, Wkv[:, cc, h * 256:h * 256 + 128], kvn[:, cc, :], start=(cc == 0), stop=(cc == 1)),
                         reads=[bWkv, bkvn], writes=[bA])
                O, bO = nog()
                S.op("act", lambda e: e.activation(out=O[:], in_=A, func=AF.Copy), reads=[bA], writes=[bO])
                S.dma("sp", knT[h, :, cs], O[:], reads=[bO], writes=[bknT])
            for sub in range(4):
                V = vs[st["iv"] % 2]
                bV = bvs[st["iv"] % 2]
                st["iv"] += 1
                for h in range(8):
                    if h % 4 == 0:
                        A, bA = nbank()
                    for cc in range(2):
                        S.op("pe", lambda e: e.matmul(A[:, (h % 4) * 128:(h % 4 + 1) * 128], kvn[:, cc, sub * 128:(sub + 1) * 128],
                                                      Wkv[:, cc, h * 256 + 128:h * 256 + 256], start=(cc == 0), stop=(cc == 1)),
                             reads=[bWkv, bkvn], writes=[bA])
                    if h % 4 == 3:
                        half = h // 4
                        S.op("act", lambda e: e.activation(out=V[:, half * 512:(half + 1) * 512], in_=A, func=AF.Copy),
                             reads=[bA], writes=[bV])
                r0 = t * 512 + sub * 128
                S.dma("sp", vm[r0:r0 + 128, :], V[:], reads=[bV], writes=[bvm])
            A, bA = nbank()
            for kc in range(16):
                S.op("pe", lambda e: e.matmul(A[0:64, :], Win[:, kc, 768:832], Xt[:, kc, :], start=(kc == 0), stop=(kc == 15)),
                     reads=[bWin, bXt], writes=[bA])
            O, bO = nog()
            rope_evac(kb, c, A, bA, RB, bRB, c["PM"], 64, tb["M_c"], tb["M_s"], bT, qa, bqa, t1, bt1, t2, bt2, O, bO)
            S.dma("sp", kpeT[:, cs], O[0:64, :], reads=[bO], writes=[bkpeT])
            for hh in range(10):
                col0 = 832 + hh * 128
                A, bA = nbank()
                for kc in range(16):
                    S.op("pe", lambda e: e.matmul(A, Win[:, kc, col0:col0 + 128], Xt[:, kc, :], start=(kc == 0), stop=(kc == 15)),
                         reads=[bWin, bXt], writes=[bA])
                S.op("act", lambda e: e.activation(out=g32[:], in_=A, func=AF.Copy), reads=[bA], writes=[bg32])
                S.op("act", lambda e: e.activation(out=sqb[:], in_=g32[:], func=AF.Square), reads=[bg32], writes=[bsqb])
                S.op("pe", lambda e: e.matmul(MS, c["ones_b"][:], sqb[:], start=True, stop=True), reads=[bsqb, c["buf"]], writes=[bMS])
                S.op("dve", lambda e: e.tensor_scalar(out=rstd[:], in0=MS, scalar1=1.0 / 128, scalar2=RMS_EPS,
                                                      op0=ALU.mult, op1=ALU.add), reads=[bMS], writes=[brstd])
                S.op("act", lambda e: e.activation(out=rstd[:], in_=rstd[:], func=AF.Sqrt), reads=[brstd], writes=[brstd])
                S.op("dve", lambda e: e.reciprocal(out=rstd[:], in_=rstd[:]), reads=[brstd], writes=[brstd])
                nc_i = 6 if hh < 8 else 7
                S.op("dve", lambda e: e.scalar_tensor_tensor(out=g32[:], in0=g32[:], scalar=ncol[:, nc_i:nc_i + 1], in1=rstd[:],
                                                             op0=ALU.mult, op1=ALU.mult), reads=[bg32, brstd, bncol], writes=[bg32])
                O, bO = nog()
                rope_evac(kb, c, g32, bg32, RB, bRB, c["PG"], 128, tb["G_c"], tb["G_s"], bT, qa, bqa, t1, bt1, t2, bt2, O, bO)
                if hh < 8:
                    S.dma("sp", gqT[hh, :, cs], O[:], reads=[bO], writes=[bgqT])
                else:
                    S.dma("sp", gkT[hh - 8, :, cs], O[:], reads=[bO], writes=[bgkT])
            for sub in range(4):
                A, bA = nbank()
                for kc in range(16):
                    S.op("pe", lambda e: e.matmul(A[:, 0:256], Xt[:, kc, sub * 128:(sub + 1) * 128], Win[:, kc, 2112:2368],
                                                  start=(kc == 0), stop=(kc == 15)), reads=[bWin, bXt], writes=[bA])
                O, bO = nog()
                S.op("act", lambda e: e.activation(out=O[:, 0:256], in_=A[:, 0:256], func=AF.Copy), reads=[bA], writes=[bO])
                r0 = t * 512 + sub * 128
                S.dma("sp", gv[r0:r0 + 128, :], O[:, 0:256], reads=[bO], writes=[bgv])
    S.barrier()


def _attn_stage(kb, c, job, tag, heads, kv_of, load_head_kv, make_terms, qloads, scale, orow0):
    nc, S = kb.nc, kb.S
    n = job["name"]
    Sn = job["S"]
    NT = Sn // 512
    oT, boT = kb.dr[f"oT_{n}"], kb.db[f"oT_{n}"]
    S.barrier()
    with ExitStack() as es:
        def sb(name, shape, dt):
            return es.enter_context(nc.sbuf_tensor(U(name), shape, dt))
        R = AttnRes(kb, es, tag)
        kv = load_head_kv(sb)
        qbufs = [[sb(f"{tag}q{j}_{i}", [pp, 512], BF16) for i in range(2)] for j, (pp, _) in enumerate(qloads)]
        bq = [Buf(), Buf()]
        om = sb(f"{tag}om", [128, 512], F32); bom = Buf()
        ob = [sb(f"{tag}ob{i}", [128, 512], BF16) for i in range(2)]
        bob = [Buf(), Buf()]
        iq = 0
        cur = None
        for h in range(heads):
            if kv_of(h) != cur:
                cur = kv_of(h)
                kv["load"](cur)
            for t in range(NT):
                i = iq % 2
                Q = [qb[i] for qb in qbufs]
                for (pp, srcfn), q in zip(qloads, Q):
                    S.dma("sp", q[:], srcfn(h)[:, t * 512:(t + 1) * 512], writes=[bq[i]])
                terms = make_terms(kv, Q, bq[i])
                attn_tile(kb, c, R, Sn, terms, kv["v"], kv["bv"], scale, om, bom)
                OB = ob[i]
                S.op("dve", lambda e: e.tensor_copy(out=OB[:], in_=om[:]), reads=[bom], writes=[bob[i]])
                S.dma("sp", oT[orow0 + h * 128:orow0 + (h + 1) * 128, t * 512:(t + 1) * 512], OB[:], reads=[bob[i]], writes=[boT])
                iq += 1
    S.barrier()


def s2_mla(kb, c, job):
    n = job["name"]
    Sn = job["S"]
    S = kb.S
    knT, qnT, qpeT, kpeT, vm = (kb.dr[f"{x}_{n}"] for x in ("knT", "qnT", "qpeT", "kpeT", "vm"))

    def load_head_kv(sb):
        kT = sb("mlkT", [128, Sn], BF16)
        kpe = sb("mlkpe", [64, Sn], BF16)
        v = sb("mlv", [128, Sn // 128, 128], BF16)
        st = {"kT": kT, "kpe": kpe, "v": v, "bk": Buf(), "bv": Buf(), "bkpe": Buf()}
        S.dma("sp", kpe[:], kpeT[:, :], writes=[st["bkpe"]])

        def load(h):
            load_kv(kb, kT, st["bk"], knT[h], v, st["bv"], vm[:, h * 128:(h + 1) * 128], Sn)
        st["load"] = load
        return st

    def make_terms(kv, Q, bQ):
        return [(lambda kc: kv["kT"][:, kc * 128:(kc + 1) * 128], Q[0][:], [kv["bk"], bQ]),
                (lambda kc: kv["kpe"][:, kc * 128:(kc + 1) * 128], Q[1][:], [kv["bkpe"], bQ])]

    _attn_stage(kb, c, job, "ml", 8, lambda h: h, load_head_kv, make_terms,
                [(128, lambda h: qnT[h]), (64, lambda h: qpeT[h])], 192 ** -0.5, 0)


def s3_gqa(kb, c, job):
    n = job["name"]
    Sn = job["S"]
    gqT, gkT, gv = (kb.dr[f"{x}_{n}"] for x in ("gqT", "gkT", "gv"))

    def load_head_kv(sb):
        kT = sb("gqkT", [128, Sn], BF16)
        v = sb("gqv", [128, Sn // 128, 128], BF16)
        st = {"kT": kT, "v": v, "bk": Buf(), "bv": Buf()}

        def load(j):
            load_kv(kb, kT, st["bk"], gkT[j], v, st["bv"], gv[:, j * 128:(j + 1) * 128], Sn)
        st["load"] = load
        return st

    def make_terms(kv, Q, bQ):
        return [(lambda kc: kv["kT"][:, kc * 128:(kc + 1) * 128], Q[0][:], [kv["bk"], bQ])]

    _attn_stage(kb, c, job, "gq", 8, lambda h: h // 4, load_head_kv, make_terms,
                [(128, lambda h: gqT[h])], 128 ** -0.5, 1024)


JOBS = [dict(name="s", S=4096, cap=512), dict(name="p", S=16384, cap=1536)]


def kernel(**inputs):
    jobs = [dict(j) for j in JOBS]
    nc, kb = build_program(jobs)
    used = set(kb.dr.keys())
    base = {}
    for name, shp in WEIGHT_SHAPES.items():
        if name in used:
            base[name] = np.ascontiguousarray(np.asarray(inputs[name], dtype=np.float32).reshape(shp))
    for name, arr in host_consts(max(j["S"] for j in jobs)).items():
        if name in used:
            base[name] = arr
    for j in jobs:
        base[f"ecs_{j['name']}"] = np.ascontiguousarray(
            np.tile((np.arange(32, dtype=np.float32) * (j["cap"] + 128))[None, :], (128, 1)))
    xp = np.asarray(inputs["x_prompt"], dtype=np.float32)[0]
    base["x_p"] = np.ascontiguousarray(xp)
    base["xT_p"] = np.ascontiguousarray(xp.T)
    xs = np.asarray(inputs["x_sample"], dtype=np.float32)
    in_maps = []
    for core in range(8):
        m = dict(base)
        m["x_s"] = np.ascontiguousarray(xs[core % 4])
        m["xT_s"] = np.ascontiguousarray(xs[core % 4].T)
        in_maps.append({k: v for k, v in m.items() if k in used})
    res = run_bass_kernel_spmd(nc, in_maps, core_ids=list(range(8)))
    y_prompt = np.asarray(res.results[0]["y_p"], dtype=np.float32)[None]
    y_sample = np.stack([np.asarray(res.results[cix]["y_s"], dtype=np.float32) for cix in range(4)], 0)
    return (y_prompt, y_sample)
```
